# Optimizing a Trainium2 kernel written in Bass

```python
import jax, jax.numpy as jnp
from jax import lax
import numpy as np

D_MODEL = 1024
BATCH = 8
SEQ = 4096
DEPTH = 1

CHUNK = 64
D_INNER = 2 * D_MODEL
SSD_HEAD_DIM = 64
SSD_HEADS = D_INNER // SSD_HEAD_DIM
SSD_GROUPS = 8
SSD_STATE = 128
SSD_CONV = 4
SSD_CONV_DIM = D_INNER + 2 * SSD_GROUPS * SSD_STATE
CONF_DIM = D_MODEL
CONF_KERNEL = 31
PEER_HEADS = 8
PEER_N_KEYS = 128
PEER_EXPERTS = PEER_N_KEYS * PEER_N_KEYS
PEER_TOPK = 16
PEER_KEY_DIM = 256
PEER_HALF = PEER_KEY_DIM // 2
PEER_TOKEN_BLOCK = 128
PLE_DIM = 256
COL_Z = D_INNER
COL_XBC = COL_Z + SSD_CONV_DIM
COL_DT = COL_XBC + SSD_HEADS
COL_GLU = COL_DT + 2 * CONF_DIM
COL_GA = COL_GLU + D_MODEL
IN_COLS = COL_GA + D_MODEL
EPS = 1e-6

kernel_name = "hybrid_ssd_conformer_peer_block"


def rms_norm(x, w):
    x32 = x.astype(jnp.float32)
    y = x32 * lax.rsqrt(jnp.mean(x32 * x32, axis=-1, keepdims=True) + EPS)
    return (y * w.astype(jnp.float32)).astype(x.dtype)


def layer_norm(x, w, b):
    x32 = x.astype(jnp.float32)
    mu = jnp.mean(x32, axis=-1, keepdims=True)
    xc = x32 - mu
    y = xc * lax.rsqrt(jnp.mean(xc * xc, axis=-1, keepdims=True) + EPS)
    return (y * w.astype(jnp.float32) + b.astype(jnp.float32)).astype(x.dtype)


def gated_group_rms_norm(y, z, w):
    yz = (y * jax.nn.silu(z)).astype(jnp.float32)
    shp = yz.shape
    yg = yz.reshape(shp[:-1] + (SSD_GROUPS, shp[-1] // SSD_GROUPS))
    yg = yg * lax.rsqrt(jnp.mean(yg * yg, axis=-1, keepdims=True) + EPS)
    return (yg.reshape(shp) * w.astype(jnp.float32)).astype(y.dtype)


def causal_depthwise_conv(x, w, b):
    width, ch = w.shape
    y = lax.conv_general_dilated(x, w[:, None, :].astype(x.dtype), window_strides=(1,),
                                 padding=[(width - 1, 0)],
                                 dimension_numbers=("NWC", "WIO", "NWC"),
                                 feature_group_count=ch)
    return y + b.astype(x.dtype)


def ssd_chunked_scan(xh, dt, a, bm, cm):
    f32 = jnp.float32
    bsz, s, h, pdim = xh.shape
    g, n = bm.shape[2], bm.shape[3]
    r = h // g
    nc = s // CHUNK
    x_dt = (xh.astype(f32) * dt[..., None]).reshape(bsz, nc, CHUNK, g, r, pdim)
    log_a = (dt * a).reshape(bsz, nc, CHUNK, g, r)
    bc_all = bm.astype(f32).reshape(bsz, nc, CHUNK, g, n)
    cc_all = cm.astype(f32).reshape(bsz, nc, CHUNK, g, n)
    xs = tuple(jnp.moveaxis(t, 1, 0) for t in (x_dt, log_a, bc_all, cc_all))
    causal = jnp.tril(jnp.ones((CHUNK, CHUNK), dtype=bool))[None, :, :, None, None]

    def step(state, inp):
        xc, lac, bc, cc = inp
        acum = jnp.cumsum(lac, axis=1)
        seg = acum[:, :, None] - acum[:, None, :]
        lmat = jnp.exp(jnp.where(causal, seg, -jnp.inf))
        cb = jnp.einsum("blgn,bsgn->blsg", cc, bc)
        y_diag = jnp.einsum("blsg,blsgr,bsgrp->blgrp", cb, lmat, xc)
        y_off = jnp.einsum("blgn,bgrpn->blgrp", cc, state) * jnp.exp(acum)[..., None]
        decay = jnp.exp(acum[:, -1:] - acum)
        new_state = (state * jnp.exp(acum[:, -1])[..., None, None]
                     + jnp.einsum("bsgn,bsgr,bsgrp->bgrpn", bc, decay, xc))
        return new_state, y_diag + y_off

    state0 = jnp.zeros((bsz, g, r, pdim, n), f32)
    _, ys = lax.scan(step, state0, xs)
    return jnp.moveaxis(ys, 0, 1).reshape(bsz, s, h, pdim)


def peer_mixer(xn, wq, keys, u_tab, v_tab):
    bsz, s, d = xn.shape
    blocks = xn.reshape(-1, PEER_TOKEN_BLOCK, d)

    def block(xb):
        t = xb.shape[0]
        q = (xb @ wq).reshape(t, PEER_HEADS, 2, PEER_HALF)
        scores = jnp.einsum("thcd,hckd->thck", q, keys.astype(xb.dtype))
        sv, si = lax.top_k(scores, PEER_TOPK)
        cand = (sv[:, :, 0, :, None] + sv[:, :, 1, None, :]).reshape(t, PEER_HEADS, PEER_TOPK * PEER_TOPK)
        best, j = lax.top_k(cand, PEER_TOPK)
        i1 = jnp.take_along_axis(si[:, :, 0], j // PEER_TOPK, axis=-1)
        i2 = jnp.take_along_axis(si[:, :, 1], j % PEER_TOPK, axis=-1)
        expert = i1 * PEER_N_KEYS + i2
        gate = jax.nn.softmax(best.astype(jnp.float32), axis=-1).astype(xb.dtype)
        u = u_tab[expert]
        act = jax.nn.gelu(jnp.einsum("thkd,td->thk", u, xb), approximate=False)
        v = v_tab[expert]
        return jnp.einsum("thk,thkd->td", gate * act, v)

    out = lax.map(block, blocks)
    return out.reshape(bsz, s, d)


def setup_inputs(seed: int = 0) -> dict:
    key = jax.random.key(seed)
    ks = jax.random.split(key, 32)
    f32 = jnp.float32
    nrm = lambda k, shape, scale: jax.random.normal(k, shape, f32) * scale
    L = DEPTH
    dt0 = jnp.exp(jax.random.uniform(ks[5], (L, SSD_HEADS), f32,
                                     minval=float(np.log(1e-3)), maxval=float(np.log(1e-1))))
    return {
        "x": nrm(ks[0], (BATCH, SEQ, D_MODEL), 1.0),
        "p": nrm(ks[1], (DEPTH, BATCH, SEQ, PLE_DIM), 1.0),
        "norm_mix_w": 1.0 + nrm(ks[2], (L, D_MODEL), 0.02),
        "w_in": nrm(ks[3], (L, D_MODEL, IN_COLS), D_MODEL ** -0.5),
        "conv_ssd_w": nrm(ks[4], (L, SSD_CONV, SSD_CONV_DIM), SSD_CONV ** -0.5),
        "conv_ssd_b": nrm(ks[6], (L, SSD_CONV_DIM), 0.02),
        "dt_bias": dt0 + jnp.log(-jnp.expm1(-dt0)),
        "a_log": jnp.log(jax.random.uniform(ks[7], (L, SSD_HEADS), f32, minval=1.0, maxval=16.0)),
        "d_skip": 1.0 + nrm(ks[8], (L, SSD_HEADS), 0.02),
        "ssd_norm_w": 1.0 + nrm(ks[9], (L, D_INNER), 0.02),
        "w_ssd_out": nrm(ks[10], (L, D_INNER, D_MODEL), D_INNER ** -0.5),
        "conv_dw_w": nrm(ks[11], (L, CONF_KERNEL, CONF_DIM), CONF_KERNEL ** -0.5),
        "conv_dw_b": nrm(ks[12], (L, CONF_DIM), 0.02),
        "conv_ln_w": 1.0 + nrm(ks[13], (L, CONF_DIM), 0.02),
        "conv_ln_b": nrm(ks[14], (L, CONF_DIM), 0.02),
        "w_conv_out": nrm(ks[15], (L, CONF_DIM, D_MODEL), CONF_DIM ** -0.5),
        "b_conv_out": nrm(ks[16], (L, D_MODEL), 0.02),
        "w_o": nrm(ks[17], (L, D_MODEL, D_MODEL), D_MODEL ** -0.5),
        "norm_ffn_w": 1.0 + nrm(ks[18], (L, D_MODEL), 0.02),
        "peer_wq": nrm(ks[19], (L, D_MODEL, PEER_HEADS * PEER_KEY_DIM), D_MODEL ** -0.5),
        "peer_keys": nrm(ks[20], (L, PEER_HEADS, 2, PEER_N_KEYS, PEER_HALF), PEER_HALF ** -0.5),
        "peer_u": nrm(ks[21], (L, PEER_EXPERTS, D_MODEL), D_MODEL ** -0.5),
        "peer_v": nrm(ks[22], (L, PEER_EXPERTS, D_MODEL), PEER_HEADS ** -0.5),
        "norm_ple_w": 1.0 + nrm(ks[23], (L, D_MODEL), 0.02),
        "w_ple_gate": nrm(ks[24], (L, D_MODEL, D_MODEL), D_MODEL ** -0.5),
        "w_ple_proj": nrm(ks[25], (L, PLE_DIM, D_MODEL), PLE_DIM ** -0.5),
        "final_norm_w": 1.0 + nrm(ks[26], (D_MODEL,), 0.02),
    }


def reference(x, p, norm_mix_w, w_in, conv_ssd_w, conv_ssd_b, dt_bias, a_log, d_skip,
              ssd_norm_w, w_ssd_out, conv_dw_w, conv_dw_b, conv_ln_w, conv_ln_b,
              w_conv_out, b_conv_out, w_o, norm_ffn_w, peer_wq, peer_keys, peer_u, peer_v,
              norm_ple_w, w_ple_gate, w_ple_proj, final_norm_w):
    f32 = jnp.float32
    bsz, s, _ = x.shape
    h = x
    for i in range(DEPTH):
        hn = rms_norm(h, norm_mix_w[i])
        proj = hn @ w_in[i]
        z = proj[..., :COL_Z]
        xbc = proj[..., COL_Z:COL_XBC]
        dt_raw = proj[..., COL_XBC:COL_DT]
        glu = proj[..., COL_DT:COL_GLU]
        gate_a = proj[..., COL_GLU:COL_GA]
        gate_b = proj[..., COL_GA:]

        xbc = jax.nn.silu(causal_depthwise_conv(xbc, conv_ssd_w[i], conv_ssd_b[i]))
        x_ssm = xbc[..., :D_INNER].reshape(bsz, s, SSD_HEADS, SSD_HEAD_DIM)
        b_ssm = xbc[..., D_INNER:D_INNER + SSD_GROUPS * SSD_STATE].reshape(bsz, s, SSD_GROUPS, SSD_STATE)
        c_ssm = xbc[..., D_INNER + SSD_GROUPS * SSD_STATE:].reshape(bsz, s, SSD_GROUPS, SSD_STATE)
        dt = jax.nn.softplus(dt_raw.astype(f32) + dt_bias[i].astype(f32))
        a = -jnp.exp(a_log[i].astype(f32))
        y_ssm = ssd_chunked_scan(x_ssm, dt, a, b_ssm, c_ssm)
        y_ssm = y_ssm + d_skip[i].astype(f32)[:, None] * x_ssm.astype(f32)
        y_ssm = y_ssm.reshape(bsz, s, D_INNER).astype(x.dtype)
        y_a = gated_group_rms_norm(y_ssm, z, ssd_norm_w[i]) @ w_ssd_out[i]

        u = glu[..., :CONF_DIM] * jax.nn.sigmoid(glu[..., CONF_DIM:])
        u = causal_depthwise_conv(u, conv_dw_w[i], conv_dw_b[i])
        u = jax.nn.silu(layer_norm(u, conv_ln_w[i], conv_ln_b[i]))
        y_b = u @ w_conv_out[i] + b_conv_out[i]

        merged = jax.nn.sigmoid(gate_a) * y_a + jax.nn.sigmoid(gate_b) * y_b
        h = h + merged @ w_o[i]

        h = h + peer_mixer(rms_norm(h, norm_ffn_w[i]), peer_wq[i], peer_keys[i], peer_u[i], peer_v[i])

        ple_gate = jax.nn.sigmoid(rms_norm(h, norm_ple_w[i]) @ w_ple_gate[i])
        h = h + ple_gate * (p[i] @ w_ple_proj[i])
    return rms_norm(h, final_norm_w)
```

```python
import numpy as np
from contextlib import ExitStack
import concourse.bass as bass
import concourse.mybir as mybir
from concourse.bass_utils import run_bass_kernel_spmd

F32 = mybir.dt.float32
BF16 = mybir.dt.bfloat16
I32 = mybir.dt.int32
U32 = mybir.dt.uint32
AF = mybir.ActivationFunctionType
ALU = mybir.AluOpType
AX = mybir.AxisListType

D = 1024
DI = 2048
INC = 10272
C_Z0, C_XBC0, C_DT0, C_GLUA, C_GLUB, C_GA, C_GB = 0, 2048, 6144, 6176, 7200, 8224, 9248
EPS = 1e-6
CV_NMIX, CV_SSDN, CV_NFFN, CV_NPLE, CV_LNW, CV_LNB, CV_CSSB, CV_CDWB, CV_CSSW, CV_CDWW, CV_N = 0, 8, 24, 32, 40, 48, 56, 88, 96, 224, 472
RV_FNW, RV_NFFN, RV_DTB, RV_ALOG, RV_DSK, RV_BCO, RV_N = 0, 1024, 2048, 2080, 2112, 2144, 3168
NWSLOT = 4


class Sched:
    def __init__(self, nc, stack, n_dma_sems=16, same_engine_sync=("act", "dve", "pool")):
        self.nc = nc
        self.names = ["pe", "act", "dve", "pool", "sp"]
        self.sem = {e: stack.enter_context(nc.semaphore("sem_" + e)) for e in self.names}
        self.cnt = {e: 0 for e in self.names}
        self.prog = {e: [] for e in self.names}
        self.waited = {e: {} for e in self.names}
        self.dsem, self.dcnt, self.dnext = {}, {}, {}
        for q in ("sp", "pool"):
            self.dsem[q] = [stack.enter_context(nc.semaphore(f"dsem_{q}{i}")) for i in range(n_dma_sems)]
            self.dcnt[q] = [0] * n_dma_sems
            self.dnext[q] = 0
        self.last_w = {}
        self.readers = {}
        self.same_engine_sync = same_engine_sync
        self.out_tokens = []

    def _need(self, e, tok):
        key, h, v, pe = tok
        if pe == e and (e == "pe" or e not in self.same_engine_sync):
            return
        if self.waited[e].get(key, 0) >= v:
            return
        self.waited[e][key] = v
        self.prog[e].append(("wait", h, v))

    def op(self, e, fn, reads=(), writes=(), dma=False, final=False):
        deps = []
        for r in reads:
            if r in self.last_w:
                deps.append(self.last_w[r])
        for w in writes:
            if w in self.last_w:
                deps.append(self.last_w[w])
            deps.extend(self.readers.get(w, {}).values())
        for tok in deps:
            self._need(e, tok)
        if dma:
            i = self.dnext[e]
            self.dnext[e] = (i + 1) % len(self.dsem[e])
            self.dcnt[e][i] += 16
            tok = (("d", e, i), self.dsem[e][i], self.dcnt[e][i], "dma")
            self.prog[e].append(("inst", fn, self.dsem[e][i], 16))
        else:
            self.cnt[e] += 1
            tok = (("e", e), self.sem[e], self.cnt[e], e)
            self.prog[e].append(("inst", fn, self.sem[e], 1))
        for r in reads:
            self.readers.setdefault(r, {})[tok[0]] = tok
        for w in writes:
            self.last_w[w] = tok
            self.readers[w] = {}
        if final:
            self.out_tokens.append(tok)
        return tok

    def emit(self):
        nc = self.nc
        for tok in self.out_tokens:
            self._need("sp", tok)
        progs = self.prog

        def run(eng, lst):
            for it in lst:
                if it[0] == "wait":
                    eng.wait_ge(it[1], it[2])
                else:
                    it[1](eng).then_inc(it[2], it[3])

        with nc.Block() as block:
            @block.tensor
            def _(eng):
                run(eng, progs["pe"])

            @block.scalar
            def _(eng):
                run(eng, progs["act"])

            @block.vector
            def _(eng):
                run(eng, progs["dve"])

            @block.gpsimd
            def _(eng):
                run(eng, progs["pool"])

            @block.sync
            def _(eng):
                run(eng, progs["sp"])


def build_nc(T, dbg=(), dbg_tile=0, same_engine_sync=("act", "dve", "pool"), wseq=None):
    NT = T // 128
    nc = bass.Bass("TRN2", target_bir_lowering=False)
    din = lambda n, s, dt=F32: nc.dram_tensor(n, list(s), dt, kind="ExternalInput").ap()
    x_d = din("x", [T, D])
    p_d = din("p", [T, 256])
    wsrc = {
        "w_in": din("w_in", [D, INC]), "w_ssd_out": din("w_ssd_out", [DI, D]),
        "w_conv_out": din("w_conv_out", [D, D]), "w_o": din("w_o", [D, D]),
        "peer_wq": din("peer_wq", [D, 2048]), "w_ple_gate": din("w_ple_gate", [D, D]),
        "w_ple_proj": din("w_ple_proj", [256, D]),
    }
    wsrc["kT"] = din("kT", [128, 2048])
    pu_d = din("peer_u", [16384, D])
    pv_d = din("peer_v", [16384, D])
    cvec_d = din("cvec", [128, CV_N])
    rowvec_d = din("rowvec", [RV_N])
    out_d = nc.dram_tensor("out", [T, D], F32, kind="ExternalOutput").ap()
    wblk = nc.dram_tensor("wblk", [48, 128, 8 * 512], BF16, kind="Internal").ap()
    uv_scr = nc.dram_tensor("uv_scr", [16384, 2048], BF16, kind="Internal").ap()
    dbg_out = {}

    with ExitStack() as st:
        S = Sched(nc, st, same_engine_sync=same_engine_sync)
        sb = lambda name, shape, dt=F32: st.enter_context(nc.sbuf_tensor("sb_" + name, list(shape), dt))
        PSB = [st.enter_context(nc.psum_tensor(f"psb{b}", [128, 512], F32)) for b in range(8)]
        PR = lambda b: ("ps", b)
        bank_ctr = [0]
        bank_lim = [6]

        def nb():
            b = bank_ctr[0] % bank_lim[0]
            bank_ctr[0] = (b + 1) % bank_lim[0]
            return b

        def mm(out, lhsT, rhs, start, stop, r, w):
            S.op("pe", lambda e: e.matmul(out, lhsT=lhsT, rhs=rhs, start=start, stop=stop), reads=r, writes=w)

        def tr(out, in_, ident, r, w):
            S.op("pe", lambda e: e.transpose(out=out, in_=in_, identity=ident), reads=r, writes=w)

        def actf(out, in_, func, r, w, **kw):
            S.op("act", lambda e: e.activation(out=out, in_=in_, func=func, **kw), reads=r, writes=w)

        def tt(eng, out, in0, in1, op, r, w):
            S.op(eng, lambda e: e.tensor_tensor(out=out, in0=in0, in1=in1, op=op), reads=r, writes=w)

        def tsc(eng, out, in0, s1, s2, op0, op1, r, w):
            if s2 is None:
                S.op(eng, lambda e: e.tensor_scalar(out=out, in0=in0, scalar1=s1, scalar2=None, op0=op0), reads=r, writes=w)
            else:
                S.op(eng, lambda e: e.tensor_scalar(out=out, in0=in0, scalar1=s1, scalar2=s2, op0=op0, op1=op1), reads=r, writes=w)

        def tss(eng, out, in_, s, op, r, w):
            S.op(eng, lambda e: e.tensor_single_scalar(out=out, in_=in_, scalar=s, op=op), reads=r, writes=w)

        def stt(eng, out, in0, scalar, in1, op0, op1, r, w):
            S.op(eng, lambda e: e.scalar_tensor_tensor(out=out, in0=in0, scalar=scalar, in1=in1, op0=op0, op1=op1), reads=r, writes=w)

        def cp(eng, out, in_, r, w):
            if eng == "act":
                S.op("act", lambda e: e.copy(out=out, in_=in_), reads=r, writes=w)
            else:
                S.op(eng, lambda e: e.tensor_copy(out=out, in_=in_), reads=r, writes=w)

        def red(eng, out, in_, op, r, w):
            S.op(eng, lambda e: e.tensor_reduce(out=out, in_=in_, axis=AX.X, op=op), reads=r, writes=w)

        def recip(out, in_, r, w):
            S.op("dve", lambda e: e.reciprocal(out=out, in_=in_), reads=r, writes=w)

        def mset(eng, ap, v, w):
            S.op(eng, lambda e: e.memset(ap, v), writes=w)

        def dma(q, out, in_, r, w, final=False):
            S.op(q, lambda e: e.dma_start(out=out, in_=in_), reads=r, writes=w, dma=True, final=final)

        def mkap(t, off, dims):
            a = t[:]
            return bass.AP(a.tensor, off, [list(a.ap[0])] + [list(d) for d in dims])

        def dump(name, ap, shape, r, dt=F32):
            if name in dbg:
                o = nc.dram_tensor("dbg_" + name, list(shape), dt, kind="ExternalOutput").ap()
                dbg_out[name] = o
                dma("sp", o, ap, r, [], final=True)

        identf = sb("identf", [128, 128]); identb = sb("identb", [128, 128], BF16)
        triu = sb("triu", [128, 128]); slt = sb("slt", [128, 128]); onesf = sb("onesf", [128, 128])
        onesb = sb("onesb", [1, 128], BF16)
        triub = sb("triub", [128, 128], BF16)
        iota16 = sb("iota16", [128, 16])
        cvec = sb("cvec", [128, CV_N]); brow = sb("brow", [128, RV_BCO])
        aneg = sb("aneg", [128, 32])
        bcob = sb("bcob", [1, D], BF16)
        state = sb("state", [128, 2048]); state_bf = sb("state_bf", [128, 2048], BF16)
        halo_x = sb("halo_x", [128, 32, 3]); rawb = [sb(f"rawb{i}", [128, 4, 131]) for i in range(2)]
        uraw = sb("uraw", [128, 8, 158])
        wbuf = [sb(f"wbuf{i}", [128, 8, 512], BF16) for i in range(NWSLOT)]
        Fb = [sb(f"F{i}", [128, 2048]) for i in range(4)]
        Hb = [sb(f"H{i}", [128, 2048], BF16) for i in range(4)]
        FR = lambda i: [("F", i, 0), ("F", i, 1)]
        HR = lambda i: [("H", i)]
        xts = [sb(f"xt{i}", [128, D]) for i in range(2)]; xs = sb("xs", [128, D], BF16); hnT = sb("hnT", [128, 8, 128], BF16)
        xsns = [sb(f"xsn{i}", [128, D], BF16) for i in range(2)]
        gbufs = [sb(f"gb{i}", [128, 2048], BF16) for i in range(8)]
        sm = sb("sm", [128, 64])
        dtt = sb("dtt", [128, 32]); la = sb("la", [128, 32]); acum = sb("acum", [128, 32]); decarg = sb("decarg", [128, 32])
        eacum = sb("eacum", [128, 32]); decay = sb("decay", [128, 32]); dA = sb("dA", [128, 32])
        xcT = sb("xcT", [128, 16, 128], BF16); BT = sb("BT", [128, 8, 128], BF16); CT = sb("CT", [128, 8, 128], BF16)
        cacc = [sb(f"cacc{i}", [128, 4, 128]) for i in range(2)]
        x_tok = sb("x_tok", [128, 2048], BF16); B_tok = sb("B_tok", [128, 1024], BF16)
        sig = sb("sig", [128, 512]); sigY = sb("sigY", [128, 512]); uconv = sb("uconv", [128, 8, 128]); uact = sb("uact", [128, 8, 128], BF16)
        lnst = sb("lnst", [128, 3, 128])
        sv = sb("sv", [128, 256]); siu = sb("siu", [128, 256], U32); sif = sb("sif", [128, 256])
        best = sb("best", [128, 128]); ju = sb("ju", [128, 128], U32); jt = sb("jt", [128, 128], U32)
        jaf = sb("jaf", [128, 128]); jbf = sb("jbf", [128, 128]); i1f = sb("i1f", [128, 128]); i2f = sb("i2f", [128, 128])
        eidxs = [sb(f"eidx{i}", [128, 128], I32) for i in range(2)]; gtes = [sb(f"gte{i}", [128, 128]) for i in range(2)]; dots = sb("dots", [128, 128]); wts = sb("wts", [128, 128])
        dg = [sb(f"dg{i}", [128, 128], BF16) for i in range(4)]
        pt = sb("pt", [128, 256]); ptb = sb("ptb", [128, 256], BF16); ptT = sb("ptT", [128, 2, 128], BF16)

        mset("pool", identf[:], 1.0, ["identf"])
        S.op("pool", lambda e: e.affine_select(out=identf[:], in_=identf[:], pattern=[[-1, 128]], compare_op=ALU.is_equal, fill=0.0, base=0, channel_multiplier=1), reads=["identf"], writes=["identf"])
        cp("dve", identb[:], identf[:], ["identf"], ["identb"])
        mset("pool", triu[:], 1.0, ["triu"])
        S.op("pool", lambda e: e.affine_select(out=triu[:], in_=triu[:], pattern=[[1, 128]], compare_op=ALU.is_ge, fill=0.0, base=0, channel_multiplier=-1), reads=["triu"], writes=["triu"])
        cp("dve", triub[:], triu[:], ["triu"], ["triub"])
        mset("pool", slt[:], 1.0, ["slt"])
        S.op("pool", lambda e: e.affine_select(out=slt[:], in_=slt[:], pattern=[[-1, 128]], compare_op=ALU.is_ge, fill=0.0, base=-1, channel_multiplier=1), reads=["slt"], writes=["slt"])
        mset("pool", onesf[:], 1.0, ["onesf"])
        mset("pool", onesb[:], 1.0, ["onesb"])
        for a in range(16):
            mset("pool", iota16[:, a:a + 1], float(a), ["iota16"])
        mset("pool", state[:], 0.0, ["state"])
        mset("pool", state_bf[:], 0.0, ["state_bf"])
        mset("pool", halo_x[:], 0.0, [("halo_x", j) for j in range(8)])
        mset("pool", uraw[:], 0.0, [("uraw", 0), ("uraw", 1)])
        dma("sp", cvec[:], cvec_d, [], ["cvec"])
        dma("sp", brow[:], rowvec_d[0:RV_BCO].partition_broadcast(128), [], ["brow"])
        dma("pool", bcob[:], rowvec_d[RV_BCO:RV_BCO + D].rearrange("(a n) -> a n", a=1), [], ["bcob"])
        actf(aneg[:], brow[:, RV_ALOG:RV_ALOG + 32], AF.Exp, ["brow"], ["aneg"])
        tss("dve", aneg[:], aneg[:], -1.0, ALU.mult, ["aneg"], ["aneg"])

        record = wseq is None
        seq = [] if record else list(wseq)
        wstate = {"issued": 0, "consumed": 0}

        blk_index = {}
        if not record:
            for spec in seq:
                if spec not in blk_index:
                    bi = len(blk_index)
                    blk_index[spec] = bi
                    n, k0, nk, c0, ncol = spec
                    src = wsrc[n][k0 * 128:(k0 + nk) * 128, c0:c0 + ncol].rearrange("(kc p) n -> p kc n", p=128)
                    dst = wblk[bi][:, 0:nk * ncol].rearrange("p (kc n) -> p kc n", kc=nk)
                    dma("pool", dst, src, [], [("blk", bi)])

        uvres = []
        for rc in range(16):
            for hf, src in enumerate((pu_d, pv_d)):
                dma("pool", uv_scr[rc * 1024:(rc + 1) * 1024, hf * 1024:(hf + 1) * 1024], src[rc * 1024:(rc + 1) * 1024, :], [], [("uv", rc, hf)])
                uvres.append(("uv", rc, hf))

        S.op("pool", lambda e: e.memset(sm[:, 60:61], 0.0), reads=uvres, writes=["uv_ready"])

        def w_issue(i):
            n, k0, nk, c0, ncol = seq[i]
            s = i % NWSLOT
            bi = blk_index[seq[i]]
            src = wblk[bi][:, 0:nk * ncol].rearrange("p (kc n) -> p kc n", kc=nk)
            dma("sp", wbuf[s][:, 0:nk, 0:ncol], src, [("blk", bi)], [("wbuf", s)])

        def wnext(expect, k0=0, ncol=512, nk=8):
            spec = (expect[0], k0, nk, expect[1], ncol)
            if record:
                seq.append(spec)
                return wbuf[0], ("wbuf", 0)
            while wstate["issued"] < min(len(seq), wstate["consumed"] + NWSLOT):
                w_issue(wstate["issued"])
                wstate["issued"] += 1
            i = wstate["consumed"]
            wstate["consumed"] += 1
            assert seq[i] == spec, (seq[i], spec)
            s = i % NWSLOT
            return wbuf[s], ("wbuf", s)

        def rmsnorm_stats(src, src_res, col, scr, scr_res):
            actf(scr, src, AF.Square, src_res, scr_res + [("sm", col)], accum_out=sm[:, col:col + 1])
            actf(sm[:, col:col + 1], sm[:, col:col + 1], AF.Sqrt, [("sm", col)], [("sm", col)], scale=1.0 / D, bias=EPS)
            recip(sm[:, col:col + 1], sm[:, col:col + 1], [("sm", col)], [("sm", col)])

        def to_featmajor(src_bf, src_res, dstT, dst_res, nchunk, scale_cols=None, eng="dve", banks=None):
            for b0 in range(0, nchunk, 8):
                b = nb() if banks is None else banks[(b0 // 8) % len(banks)]
                pv = PSB[b][:].bitcast(BF16)
                n = min(8, nchunk - b0)
                for c in range(n):
                    tr(pv[:, c * 128:(c + 1) * 128], src_bf[:, (b0 + c) * 128:(b0 + c + 1) * 128], identb[:], src_res + ["identb"], [PR(b)])
                pv3 = pv[:, 0:n * 128].rearrange("p (a b) -> p a b", a=n)
                if scale_cols is None:
                    cp(eng, dstT[:, b0:b0 + n, :], pv3, [PR(b)], dst_res)
                else:
                    sc_ap = cvec[:, scale_cols + b0:scale_cols + b0 + n].unsqueeze(2).to_broadcast([128, n, 128])
                    tt("dve", dstT[:, b0:b0 + n, :], pv3, sc_ap, ALU.mult, [PR(b), "cvec"], dst_res)

        def mixer(ti):
            t0 = ti * 128
            par = ti % 2
            xt, eidx, gte, xsn = xts[par], eidxs[par], gtes[par], xsns[par]
            XT, EI, GT, XS = ("xt", par), ("eidx", par), ("gte", par), ("xsn", par)
            dbg_on = (ti == dbg_tile)
            D_ = (lambda name, ap, shape, r, dt=F32: dump(name, ap, shape, r, dt)) if dbg_on else (lambda *a, **k: None)
            dma("sp", xt[:], x_d[t0:t0 + 128, :], [], [XT])
            rmsnorm_stats(xt[:], [XT], 0, xs[:], ["xs"])
            actf(xs[:], xt[:], AF.Copy, [XT, ("sm", 0)], ["xs"], scale=sm[:, 0:1])
            to_featmajor(xs, ["xs"], hnT, ["hnT"], 8, scale_cols=CV_NMIX)
            D_("hnT", hnT[:], [128, 8, 128], ["hnT"], BF16)

            flags = {'y': False}
            bank_lim[0] = 4

            def ythread():
                for jj in range(2):
                    ba, bg = 4, 5
                    yield
                    wa, wra = wnext(("w_in", C_GLUA + 512 * jj))
                    for q in range(4):
                        for kc in range(8):
                            mm(PSB[ba][:, q * 128:(q + 1) * 128], wa[:, kc, q * 128:(q + 1) * 128], hnT[:, kc, :], kc == 0, kc == 7, [wra, "hnT"], [PR(ba)])
                    yield
                    wg, wrg = wnext(("w_in", C_GLUB + 512 * jj))
                    for q in range(4):
                        for kc in range(8):
                            mm(PSB[bg][:, q * 128:(q + 1) * 128], wg[:, kc, q * 128:(q + 1) * 128], hnT[:, kc, :], kc == 0, kc == 7, [wrg, "hnT"], [PR(bg)])
                    actf(sigY[:], PSB[bg][:], AF.Sigmoid, [PR(bg)], ["sigY"])
                    tt("dve", uraw[:, 4 * jj:4 * jj + 4, 30:158], PSB[ba][:].rearrange("p (a b) -> p a b", a=4), sigY[:].rearrange("p (a b) -> p a b", a=4), ALU.mult, [PR(ba), "sigY"], [("uraw", jj)])

                for k in range(31):
                    yield
                    for c in range(8):
                        ur = ("uraw", c // 4)
                        wk = cvec[:, CV_CDWW + c * 31 + k:CV_CDWW + c * 31 + k + 1]
                        if k == 0:
                            tsc("dve", uconv[:, c, :], uraw[:, c, 0:128], wk, cvec[:, CV_CDWB + c:CV_CDWB + c + 1], ALU.mult, ALU.add, [ur, "cvec"], [("uconv", c)])
                        else:
                            stt("dve", uconv[:, c, :], uraw[:, c, k:k + 128], wk, uconv[:, c, :], ALU.mult, ALU.add, [ur, "cvec", ("uconv", c)], [("uconv", c)])
                for jj in range(2):
                    cp("act", uraw[:, 4 * jj:4 * jj + 4, 0:30], uraw[:, 4 * jj:4 * jj + 4, 128:158], [("uraw", jj)], [("uraw", jj)])
                ucr = [("uconv", c) for c in range(8)]
                D_("uraw", uraw[:], [128, 8, 158], [("uraw", 0), ("uraw", 1)])
                D_("uconv", uconv[:], [128, 8, 128], ucr)
                yield
                b = 4
                for c in range(8):
                    mm(PSB[b][:, 0:128], onesf[:], uconv[:, c, :], c == 0, c == 7, ["onesf", ("uconv", c)], [PR(b)])
                sq3 = sigY[:].rearrange("p (a b) -> p a b", a=4)
                for h2 in range(2):
                    actf(sq3, uconv[:, 4 * h2:4 * h2 + 4, :], AF.Square, ucr[4 * h2:4 * h2 + 4], ["sigY"])
                    for q in range(4):
                        c = 4 * h2 + q
                        mm(PSB[b][:, 128:256], onesf[:], sq3[:, q, :], c == 0, c == 7, ["onesf", "sigY"], [PR(b)])
                tss("dve", lnst[:, 0, :], PSB[b][:, 0:128], 1.0 / D, ALU.mult, [PR(b)], ["lnst"])
                tss("dve", lnst[:, 1, :], PSB[b][:, 128:256], 1.0 / D, ALU.mult, [PR(b)], ["lnst"])
                tt("dve", lnst[:, 2, :], lnst[:, 0, :], lnst[:, 0, :], ALU.mult, ["lnst"], ["lnst"])
                tt("dve", lnst[:, 1, :], lnst[:, 1, :], lnst[:, 2, :], ALU.subtract, ["lnst"], ["lnst"])
                actf(lnst[:, 1, :], lnst[:, 1, :], AF.Sqrt, ["lnst"], ["lnst"], bias=EPS)
                recip(lnst[:, 1, :], lnst[:, 1, :], ["lnst"], ["lnst"])
                un = uconv
                yield
                tt("dve", un[:], uconv[:], lnst[:, 0:1, :].to_broadcast([128, 8, 128]), ALU.subtract, ucr + ["lnst"], ucr)
                tt("dve", un[:], un[:], lnst[:, 1:2, :].to_broadcast([128, 8, 128]), ALU.mult, ucr + ["lnst"], ucr)
                yield
                for c in range(8):
                    actf(uact[:, c, :], un[:, c, :], AF.Silu, [("uconv", c), "cvec"], ["uact"], scale=cvec[:, CV_LNW + c:CV_LNW + c + 1], bias=cvec[:, CV_LNB + c:CV_LNB + c + 1])
                D_("uact", uact[:], [128, 8, 128], ["uact"], BF16)


                flags['y'] = True

            def xthread():
                for j in range(8):
                    yield
                    wb, wr = wnext(("w_in", C_XBC0 + 512 * j))
                    b = nb()
                    for q in range(4):
                        for kc in range(8):
                            mm(PSB[b][:, q * 128:(q + 1) * 128], wb[:, kc, q * 128:(q + 1) * 128], hnT[:, kc, :], kc == 0, kc == 7, [wr, "hnT"], [PR(b)])
                    rb, rbr = rawb[j % 2], ("rawb", j % 2)
                    cp("act", rb[:, :, 3:131], PSB[b][:].rearrange("p (a b) -> p a b", a=4), [PR(b)], [rbr])
                    cp("act", rb[:, :, 0:3], halo_x[:, 4 * j:4 * j + 4, :], [("halo_x", j)], [rbr])
                    ca = cacc[j % 2]
                    car = ("cacc", j % 2)
                    for k in range(4):
                        for q in range(4):
                            cc = 4 * j + q
                            wk = cvec[:, CV_CSSW + cc * 4 + k:CV_CSSW + cc * 4 + k + 1]
                            if k == 0:
                                tsc("dve", ca[:, q, :], rb[:, q, 0:128], wk, cvec[:, CV_CSSB + cc:CV_CSSB + cc + 1], ALU.mult, ALU.add, [rbr, "cvec"], [(car, q)])
                            else:
                                stt("dve", ca[:, q, :], rb[:, q, k:k + 128], wk, ca[:, q, :], ALU.mult, ALU.add, [rbr, "cvec", (car, q)], [(car, q)])
                    cp("act", halo_x[:, 4 * j:4 * j + 4, :], rb[:, :, 128:131], [rbr], [("halo_x", j)])
                    car = [(car, q) for q in range(4)]
                    if j < 4:
                        dst, dres = xcT[:, 4 * j:4 * j + 4, :], ["xcT"]
                    elif j < 6:
                        dst, dres = BT[:, 4 * (j - 4):4 * (j - 4) + 4, :], ["BT"]
                    else:
                        dst, dres = CT[:, 4 * (j - 6):4 * (j - 6) + 4, :], ["CT"]
                    actf(dst, ca[:], AF.Silu, car, dres)
                D_("xcT", xcT[:], [128, 16, 128], ["xcT"], BF16)
                D_("BT", BT[:], [128, 8, 128], ["BT"], BF16)
                D_("CT", CT[:], [128, 8, 128], ["CT"], BF16)

                yield
                wb, wr = wnext(("w_in", C_DT0), ncol=32)
                b = nb()
                for kc in range(8):
                    mm(PSB[b][:, 0:32], hnT[:, kc, :], wb[:, kc, 0:32], kc == 0, kc == 7, [wr, "hnT"], [PR(b)])
                tt("dve", dtt[:], PSB[b][:, 0:32], brow[:, RV_DTB:RV_DTB + 32], ALU.add, [PR(b), "brow"], ["dtt"])
                actf(dtt[:], dtt[:], AF.Exp, ["dtt"], ["dtt"])
                actf(dtt[:], dtt[:], AF.Ln, ["dtt"], ["dtt"], bias=1.0)
                tt("dve", la[:], dtt[:], aneg[:], ALU.mult, ["dtt", "aneg"], ["la"])
                D_("dt", dtt[:], [128, 32], ["dtt"])


                to_featmajor(xcT[:].rearrange("p a b -> p (a b)"), ["xcT"], x_tok[:].rearrange("p (a b) -> p a b", a=16), ["x_tok"], 16, eng="act")
                to_featmajor(BT[:].rearrange("p a b -> p (a b)"), ["BT"], B_tok[:].rearrange("p (a b) -> p a b", a=8), ["B_tok"], 8, eng="dve")
                yield
                b = nb()
                mm(PSB[b][:, 0:32], triu[:], la[:], True, True, ["triu", "la"], [PR(b)])
                mm(PSB[b][:, 32:64], onesf[:], la[:], True, True, ["onesf", "la"], [PR(b)])
                cp("dve", acum[:], PSB[b][:, 0:32], [PR(b)], ["acum"])
                tt("dve", decarg[:], PSB[b][:, 32:64], acum[:], ALU.subtract, [PR(b), "acum"], ["decarg"])
                actf(eacum[:], acum[:], AF.Exp, ["acum"], ["eacum"])
                actf(decay[:], decarg[:], AF.Exp, ["decarg"], ["decay"])
                actf(dA[:], PSB[b][:, 32:64], AF.Exp, [PR(b)], ["dA"])
                yield
                cbm = Fb[2][:, 0:1024].rearrange("p (a b) -> p a b", a=8)
                for gb_ in range(2):
                    b = gb_
                    for g4 in range(4):
                        g = 4 * gb_ + g4
                        mm(PSB[b][:, g4 * 128:(g4 + 1) * 128], BT[:, g, :], CT[:, g, :], True, True, ["BT", "CT"], [PR(b)])
                    tt("dve", cbm[:, 4 * gb_:4 * gb_ + 4, :], PSB[b][:].rearrange("p (a b) -> p a b", a=4), triu[:].unsqueeze(1).to_broadcast([128, 4, 128]), ALU.mult, [PR(b), "triu"], [("F", 2, 0)])
                yield
                xdt, xdd, xsk = Hb[1], Hb[2], Hb[3]
                x3 = x_tok[:].rearrange("p (h d) -> p h d", h=32)
                tt("dve", xdt[:].rearrange("p (h d) -> p h d", h=32), x3, dtt[:].unsqueeze(2).to_broadcast([128, 32, 64]), ALU.mult, ["x_tok", "dtt"], HR(1))
                tt("dve", xdd[:].rearrange("p (h d) -> p h d", h=32), xdt[:].rearrange("p (h d) -> p h d", h=32), decay[:].unsqueeze(2).to_broadcast([128, 32, 64]), ALU.mult, HR(1) + ["decay"], HR(2))
                tt("dve", xsk[:].rearrange("p (h d) -> p h d", h=32), x3, brow[:, RV_DSK:RV_DSK + 32].unsqueeze(2).to_broadcast([128, 32, 64]), ALU.mult, ["x_tok", "brow"], HR(3))
                Gt, Et, Mt = Fb[0], Fb[1], Hb[0]
                S.op("dve", lambda e: e.memset(sm[:, 63:64], 0.0), reads=[], writes=HR(0) + [("Hm", 0), ("Hm", 1)])
                yield
                for qq in range(4):
                    ph = qq % 2
                    Gq = Gt[:].bitcast(BF16)[:, ph * 2048:ph * 2048 + 1024]
                    Eq = Et[:, ph * 1024:(ph + 1) * 1024]
                    Mq = Mt[:, ph * 1024:(ph + 1) * 1024]
                    tt("dve", Gq.rearrange("p (a b) -> p a b", a=8), slt[:].unsqueeze(1).to_broadcast([128, 8, 128]), la[:, 8 * qq:8 * qq + 8].unsqueeze(2).to_broadcast([128, 8, 128]), ALU.mult, ["slt", "la"], [("F", 0, ph)])
                    for i in range(8):
                        b = i // 4
                        mm(PSB[b][:, (i % 4) * 128:(i % 4 + 1) * 128], Gq[:, i * 128:(i + 1) * 128], triub[:], True, True, [("F", 0, ph), "triub"], [PR(b)])
                    for b in range(2):
                        actf(Eq[:, b * 512:(b + 1) * 512], PSB[b][:], AF.Exp, [PR(b)], [("F", 1, ph)])
                    yield
                    Ev = mkap(Et, ph * 1024, [[512, 2], [128, 4], [1, 128]])
                    Mv = mkap(Mt, ph * 1024, [[512, 2], [128, 4], [1, 128]])
                    cbv = mkap(Fb[2], 2 * qq * 128, [[128, 2], [0, 4], [1, 128]])
                    tt("dve", Mv, Ev, cbv, ALU.mult, [("F", 1, ph), ("F", 2, 0)], [("Hm", ph)])
                    b = 2
                    mm(PSB[b][:], identb[:], xsk[:, qq * 512:(qq + 1) * 512], True, False, ["identb"] + HR(3), [PR(b)])
                    for i8 in range(8):
                        h = 8 * qq + i8
                        mm(PSB[b][:, i8 * 64:(i8 + 1) * 64], Mq[:, i8 * 128:(i8 + 1) * 128], xdt[:, h * 64:(h + 1) * 64], False, i8 == 7, [("Hm", ph)] + HR(1), [PR(b)])
                    cp("act", Fb[3][:, qq * 512:(qq + 1) * 512], PSB[b][:], [PR(b)], [("F", 3, qq // 2)])
                    yield
                S.op("dve", lambda e: e.memset(sm[:, 63:64], 0.0), reads=[("Hm", 0), ("Hm", 1)], writes=HR(0))
                yv = Fb[3]
                for b in range(4):
                    yield
                    for g2 in range(2):
                        g = 2 * b + g2
                        mm(PSB[b][:, g2 * 256:(g2 + 1) * 256], CT[:, g, :], state_bf[:, g * 256:(g + 1) * 256], True, True, ["CT", "state_bf"], [PR(b)])
                    tt("dve", sig[:].rearrange("p (h d) -> p h d", h=8), PSB[b][:].rearrange("p (h d) -> p h d", h=8), eacum[:, 8 * b:8 * b + 8].unsqueeze(2).to_broadcast([128, 8, 64]), ALU.mult, [PR(b), "eacum"], ["sig"])
                    tt("dve", yv[:, b * 512:(b + 1) * 512], yv[:, b * 512:(b + 1) * 512], sig[:], ALU.add, [("F", 3, b // 2), "sig"], [("F", 3, b // 2)])
                tt("dve", state[:].rearrange("p (h d) -> p h d", h=32), state[:].rearrange("p (h d) -> p h d", h=32), dA[:].unsqueeze(2).to_broadcast([128, 32, 64]), ALU.mult, ["state", "dA"], ["state"])
                for b in range(4):
                    yield
                    for g2 in range(2):
                        g = 2 * b + g2
                        mm(PSB[b][:, g2 * 256:(g2 + 1) * 256], B_tok[:, g * 128:(g + 1) * 128], xdd[:, g * 256:(g + 1) * 256], True, True, ["B_tok"] + HR(2), [PR(b)])
                    tt("dve", state[:, b * 512:(b + 1) * 512], state[:, b * 512:(b + 1) * 512], PSB[b][:], ALU.add, ["state", PR(b)], ["state"])
                cp("act", state_bf[:], state[:], ["state"], ["state_bf"])
                D_("yssm", yv[:], [128, 2048], FR(3))

                yz = Fb[2]
                for j in range(4):
                    yield
                    wb, wr = wnext(("w_in", C_Z0 + 512 * j))
                    b = nb()
                    for kc in range(8):
                        mm(PSB[b][:], hnT[:, kc, :], wb[:, kc, :], kc == 0, kc == 7, [wr, "hnT"], [PR(b)])
                    actf(sig[:], PSB[b][:], AF.Silu, [PR(b)], ["sig"])
                    tt("dve", yz[:, j * 512:(j + 1) * 512], yv[:, j * 512:(j + 1) * 512], sig[:], ALU.mult, [("F", 3, j // 2), "sig"], [("F", 2, j // 2)])
                yield
                actf(Fb[1][:], yz[:], AF.Square, FR(2), FR(1))
                red("dve", sm[:, 8:16], Fb[1][:].rearrange("p (g d) -> p g d", g=8), ALU.add, FR(1), [("sm", 8)])
                actf(sm[:, 8:16], sm[:, 8:16], AF.Sqrt, [("sm", 8)], [("sm", 8)], scale=1.0 / 256, bias=EPS)
                recip(sm[:, 8:16], sm[:, 8:16], [("sm", 8)], [("sm", 8)])
                yield
                yn = Hb[1]
                tt("dve", yn[:].rearrange("p (g d) -> p g d", g=8), yz[:].rearrange("p (g d) -> p g d", g=8), sm[:, 8:16].unsqueeze(2).to_broadcast([128, 8, 256]), ALU.mult, FR(2) + [("sm", 8)], HR(1))
                ynT = Hb[2][:].rearrange("p (a b) -> p a b", a=16)
                to_featmajor(yn, HR(1), ynT, HR(2), 16, scale_cols=CV_SSDN)

                while not flags['y']:
                    yield
                bank_lim[0] = 6

            yield from inter2(ythread(), xthread())
            ynT = Hb[2][:].rearrange("p (a b) -> p a b", a=16)
            bya = [nb(), nb()]
            for j in range(2):
                for kh in range(2):
                    yield
                    wb, wr = wnext(("w_ssd_out", 512 * j), k0=8 * kh)
                    for kc in range(8):
                        mm(PSB[bya[j]][:], ynT[:, 8 * kh + kc, :], wb[:, kc, :], kh == 0 and kc == 0, kh == 1 and kc == 7, HR(2) + [wr], [PR(bya[j])])
            byb = [nb(), nb()]
            for j in range(2):
                yield
                wb, wr = wnext(("w_conv_out", 512 * j))
                for c in range(8):
                    mm(PSB[byb[j]][:], uact[:, c, :], wb[:, c, :], c == 0, False, ["uact", wr], [PR(byb[j])])
                mm(PSB[byb[j]][:], onesb[0:1, :], bcob[0:1, j * 512:(j + 1) * 512], False, True, ["onesb", "bcob"], [PR(byb[j])])
            m1 = Fb[0]
            for br, (c0, banks) in enumerate(((C_GA, bya), (C_GB, byb))):
                for j in range(2):
                    yield
                    wb, wr = wnext(("w_in", c0 + 512 * j))
                    b = nb()
                    for kc in range(8):
                        mm(PSB[b][:], hnT[:, kc, :], wb[:, kc, :], kc == 0, kc == 7, [wr, "hnT"], [PR(b)])
                    actf(sig[:], PSB[b][:], AF.Sigmoid, [PR(b)], ["sig"])
                    tt("dve", m1[:, br * 1024 + j * 512:br * 1024 + (j + 1) * 512], PSB[banks[j]][:], sig[:], ALU.mult, [PR(banks[j]), "sig"], [("F", 0, br)])
            mg = Hb[3]
            tt("dve", mg[:, 0:1024], m1[:, 0:1024], m1[:, 1024:2048], ALU.add, FR(0), HR(3))
            D_("merged", mg[:, 0:1024], [128, 1024], HR(3), BF16)
            mgT = Hb[0][:, 0:1024].rearrange("p (a b) -> p a b", a=8)
            to_featmajor(mg, HR(3), mgT, HR(0), 8, eng="act")
            for j in range(2):
                yield
                wb, wr = wnext(("w_o", 512 * j))
                b = nb()
                for kc in range(8):
                    mm(PSB[b][:], mgT[:, kc, :], wb[:, kc, :], kc == 0, kc == 7, HR(0) + [wr], [PR(b)])
                tt("dve", xt[:, j * 512:(j + 1) * 512], xt[:, j * 512:(j + 1) * 512], PSB[b][:], ALU.add, [XT, PR(b)], [XT])
            D_("h1", xt[:], [128, 1024], [XT])

            rmsnorm_stats(xt[:], [XT], 1, xsn[:], [XS])
            stt("dve", xsn[:], xt[:], sm[:, 1:2], brow[:, RV_NFFN:RV_NFFN + 1024], ALU.mult, ALU.mult, [XT, ("sm", 1), "brow"], [XS])
            xnT = Hb[1][:, 0:1024].rearrange("p (a b) -> p a b", a=8)
            to_featmajor(xsn, [XS], xnT, HR(1), 8, eng="act")
            qT = Hb[2][:].rearrange("p (a b) -> p a b", a=16)
            for j in range(4):
                yield
                wb, wr = wnext(("peer_wq", 512 * j))
                b = nb()
                for q in range(4):
                    for kc in range(8):
                        mm(PSB[b][:, q * 128:(q + 1) * 128], wb[:, kc, q * 128:(q + 1) * 128], xnT[:, kc, :], kc == 0, kc == 7, [wr] + HR(1), [PR(b)])
                cp("act", qT[:, 4 * j:4 * j + 4, :], PSB[b][:].rearrange("p (a b) -> p a b", a=4), [PR(b)], HR(2))
            sc = Fb[2]
            for j in range(4):
                yield
                wkb, wkr = wnext(("kT", 512 * j), nk=1)
                b = nb()
                for q in range(4):
                    hc = 4 * j + q
                    mm(PSB[b][:, q * 128:(q + 1) * 128], qT[:, hc, :], wkb[:, 0, q * 128:(q + 1) * 128], True, True, HR(2) + [wkr], [PR(b)])
                cp("act", sc[:, j * 512:(j + 1) * 512], PSB[b][:], [PR(b)], [("F", 2, j // 2)])
            D_("scores", sc[:], [128, 2048], FR(2))
            sc2 = Fb[3]

            def top16_batch(items, n):
                def R(kind, key):
                    return [(kind, key)]
                S.op("dve", lambda e: e.memset(sm[:, 62:63], 0.0), reads=[], writes=FR(3) + [("tks2", k[4]) for k in items])
                for it, (src, src_res, vals, idxs, key) in enumerate(items):
                    S.op("dve", (lambda src, vals: lambda e: e.max(out=vals[:, 0:8], in_=src))(src, vals), reads=src_res, writes=R("tkv0", key))
                yield
                for it, (src, src_res, vals, idxs, key) in enumerate(items):
                    S.op("dve", (lambda src, vals, idxs: lambda e: e.max_index(out=idxs[:, 0:8], in_max=vals[:, 0:8], in_values=src))(src, vals, idxs), reads=src_res + R("tkv0", key), writes=R("tki0", key))
                yield
                for it, (src, src_res, vals, idxs, key) in enumerate(items):
                    s2 = sc2[:, it * n:(it + 1) * n]
                    S.op("dve", (lambda src, vals, s2: lambda e: e.match_replace(out=s2, in_to_replace=vals[:, 0:8], in_values=src, imm_value=-1e30))(src, vals, s2), reads=src_res + R("tkv0", key), writes=R("tks2", key))
                yield
                for it, (src, src_res, vals, idxs, key) in enumerate(items):
                    s2 = sc2[:, it * n:(it + 1) * n]
                    S.op("dve", (lambda vals, s2: lambda e: e.max(out=vals[:, 8:16], in_=s2))(vals, s2), reads=R("tks2", key), writes=R("tkv1", key))
                yield
                for it, (src, src_res, vals, idxs, key) in enumerate(items):
                    s2 = sc2[:, it * n:(it + 1) * n]
                    S.op("dve", (lambda vals, idxs, s2: lambda e: e.max_index(out=idxs[:, 8:16], in_max=vals[:, 8:16], in_values=s2))(vals, idxs, s2), reads=R("tks2", key) + R("tkv1", key), writes=R("tki1", key))
                S.op("dve", lambda e: e.memset(sm[:, 62:63], 0.0), reads=[("tks2", k[4]) for k in items], writes=FR(3))
                yield

            items1 = [(sc[:, hc * 128:(hc + 1) * 128], [("F", 2, hc // 8)], sv[:, hc * 16:(hc + 1) * 16], siu[:, hc * 16:(hc + 1) * 16], ("a", hc)) for hc in range(16)]
            yield from top16_batch(items1, 128)
            SVR = [(k, ("a", hc)) for hc in range(16) for k in ("tkv0", "tkv1")]
            SIR = [(k, ("a", hc)) for hc in range(16) for k in ("tki0", "tki1")]
            cp("dve", sif[:], siu[:], SIR, ["sif"])
            cand = Fb[1]
            c_out = mkap(cand, 0, [[256, 8], [16, 16], [1, 16]])
            c_in0 = mkap(sv, 0, [[32, 8], [1, 16], [0, 16]])
            c_in1 = mkap(sv, 16, [[32, 8], [0, 16], [1, 16]])
            tt("dve", c_out, c_in0, c_in1, ALU.add, SVR, FR(1))
            items2 = [(cand[:, h * 256:(h + 1) * 256], [("F", 1, h // 4)], best[:, h * 16:(h + 1) * 16], ju[:, h * 16:(h + 1) * 16], ("b", h)) for h in range(8)]
            yield from top16_batch(items2, 256)
            BSR = [(k, ("b", h)) for h in range(8) for k in ("tkv0", "tkv1")]
            JUR = [(k, ("b", h)) for h in range(8) for k in ("tki0", "tki1")]
            tss("dve", jt[:], ju[:], 15, ALU.bitwise_and, JUR, ["jt"])
            cp("dve", jbf[:], jt[:], ["jt"], ["jbf"])
            tss("dve", jt[:], ju[:], 4, ALU.logical_shift_right, JUR, ["jt"])
            cp("dve", jaf[:], jt[:], ["jt"], ["jaf"])
            oh = Fb[0]
            for half, (jsel, dst) in enumerate(((jaf, i1f), (jbf, i2f))):
                oh4 = mkap(oh, 0, [[256, 8], [16, 16], [1, 16]])
                io4 = mkap(iota16, 0, [[0, 8], [0, 16], [1, 16]])
                js4 = mkap(jsel, 0, [[16, 8], [1, 16], [0, 16]])
                si4 = mkap(sif, 16 * half, [[32, 8], [0, 16], [1, 16]])
                tt("dve", oh4, io4, js4, ALU.is_equal, ["iota16", "jaf", "jbf"], FR(0))
                tt("dve", oh4, oh4, si4, ALU.mult, FR(0) + ["sif"], FR(0))
                red("dve", dst[:], oh[:].rearrange("p (a b) -> p a b", b=16), ALU.add, FR(0), ["i1f" if half == 0 else "i2f"])
            stt("dve", eidx[:], i1f[:], 128.0, i2f[:], ALU.mult, ALU.add, ["i1f", "i2f"], [EI])
            D_(EI, eidx[:], [128, 128], [EI], I32)
            b3 = best[:].rearrange("p (h k) -> p h k", h=8)
            tt("dve", gte[:].rearrange("p (h k) -> p h k", h=8), b3, mkap(best, 0, [[16, 8], [0, 16]]), ALU.subtract, BSR, [GT])
            actf(gte[:], gte[:], AF.Exp, [GT], [GT])
            red("dve", sm[:, 16:24], gte[:].rearrange("p (h k) -> p h k", h=8), ALU.add, [GT], [("sm", 16)])
            recip(sm[:, 16:24], sm[:, 16:24], [("sm", 16)], [("sm", 16)])
            tt("dve", gte[:].rearrange("p (h k) -> p h k", h=8), gte[:].rearrange("p (h k) -> p h k", h=8), sm[:, 16:24].unsqueeze(2).to_broadcast([128, 8, 16]), ALU.mult, [GT, ("sm", 16)], [GT])
            D_("gate", gte[:], [128, 128], [GT])

            yield

        def gather(ti):
            t0 = ti * 128
            par = ti % 2
            xt, eidx, gte, xsn = xts[par], eidxs[par], gtes[par], xsns[par]
            XT, EI, GT, XS = ("xt", par), ("eidx", par), ("gte", par), ("xsn", par)
            dbg_on = (ti == dbg_tile)
            D_ = (lambda name, ap, shape, r, dt=F32: dump(name, ap, shape, r, dt)) if dbg_on else (lambda *a, **k: None)
            gs = [(gbufs[i][:], ("gb", i)) for i in range(len(gbufs))]
            NGS = len(gs)
            bv = [6, 7]
            for grp in range(32):
                gl = []
                for s8 in range(4):
                    slot = grp * 4 + s8
                    gbuf, gres = gs[slot % NGS]
                    gl.append((slot, gbuf, gres))
                    if s8 % 2 == 1:
                        yield
                    S.op("pool", (lambda gbuf, slot: lambda e: e.indirect_dma_start(out=gbuf, out_offset=None, in_=uv_scr, in_offset=bass.IndirectOffsetOnAxis(ap=eidx[:, slot:slot + 1], axis=0)))(gbuf, slot), reads=[EI, "uv_ready"], writes=[gres], dma=True)
                    S.op("dve", (lambda gbuf, slot: lambda e: e.scalar_tensor_tensor(out=gbuf[:, 0:1024], in0=gbuf[:, 0:1024], scalar=1.0, in1=xsn[:], op0=ALU.mult, op1=ALU.mult, accum_out=dots[:, slot:slot + 1]))(gbuf, slot), reads=[gres, XS], writes=[(gres, "u"), ("dots", slot)])
                g8 = slice(grp * 4, grp * 4 + 4)
                actf(wts[:, g8], dots[:, g8], AF.Gelu, [("dots", grp * 4 + q) for q in range(4)], [("wts", grp)])
                tt("dve", wts[:, g8], wts[:, g8], gte[:, g8], ALU.mult, [("wts", grp), GT], [("wts", grp)])
                yield
                for slot, gbuf, gres in gl:
                    dgt = dg[slot % 4]
                    dres = ("dg", slot % 4)
                    actf(dgt[:], identb[:], AF.Copy, ["identb", ("wts", grp)], [dres], scale=wts[:, slot:slot + 1])
                    for j in range(2):
                        mm(PSB[bv[j]][:], dgt[:], gbuf[:, 1024 + 512 * j:1024 + 512 * (j + 1)], slot == 0, slot == 127, [dres, gres], [PR(bv[j])])
            D_("wts", wts[:], [128, 128], [("wts", g) for g in range(32)])
            for j in range(2):
                tt("dve", xt[:, j * 512:(j + 1) * 512], xt[:, j * 512:(j + 1) * 512], PSB[bv[j]][:], ALU.add, [XT, PR(bv[j])], [XT])
            D_("h2", xt[:], [128, 1024], [XT])


            yield

        def fstage(ti):
            t0 = ti * 128
            par = ti % 2
            xt, eidx, gte, xsn = xts[par], eidxs[par], gtes[par], xsns[par]
            XT, EI, GT, XS = ("xt", par), ("eidx", par), ("gte", par), ("xsn", par)
            dbg_on = (ti == dbg_tile)
            D_ = (lambda name, ap, shape, r, dt=F32: dump(name, ap, shape, r, dt)) if dbg_on else (lambda *a, **k: None)
            G0, G1, G2 = ("gb", 0), ("gb", 1), ("gb", 2)
            xsF = gbufs[0][:, 0:1024]
            hpT = gbufs[0][:, 1024:2048].rearrange("p (a b) -> p a b", a=8)
            sigF = gbufs[1][:].bitcast(F32)[:, 0:512]
            otF = gbufs[2][:].bitcast(F32)
            rmsnorm_stats(xt[:], [XT], 2, xsF, [G0])
            actf(xsF, xt[:], AF.Copy, [XT, ("sm", 2)], [G0], scale=sm[:, 2:3])
            to_featmajor(xsF, [G0], hpT, [G0], 8, scale_cols=CV_NPLE, banks=[6])
            dma("sp", pt[:], p_d[t0:t0 + 128, :], [], ["pt"])
            cp("act", ptb[:], pt[:], ["pt"], ["ptb"])
            to_featmajor(ptb, ["ptb"], ptT, ["ptT"], 2, eng="act", banks=[7])
            for j in range(2):
                yield
                wb, wr = wnext(("w_ple_gate", 512 * j))
                b = 6
                for kc in range(8):
                    mm(PSB[b][:], hpT[:, kc, :], wb[:, kc, :], kc == 0, kc == 7, [G0, wr], [PR(b)])
                actf(sigF, PSB[b][:], AF.Sigmoid, [PR(b)], [G1])
                yield
                wpp, wrp = wnext(("w_ple_proj", 512 * j), nk=2)
                b2 = 7
                for kc in range(2):
                    mm(PSB[b2][:], ptT[:, kc, :], wpp[:, kc, :], kc == 0, kc == 1, ["ptT", wrp], [PR(b2)])
                tt("dve", sigF, sigF, PSB[b2][:], ALU.mult, [G1, PR(b2)], [G1])
                tt("dve", xt[:, j * 512:(j + 1) * 512], xt[:, j * 512:(j + 1) * 512], sigF, ALU.add, [XT, G1], [XT])
            yield
            rmsnorm_stats(xt[:], [XT], 3, xsF, [G0])
            stt("dve", otF, xt[:], sm[:, 3:4], brow[:, RV_FNW:RV_FNW + 1024], ALU.mult, ALU.mult, [XT, ("sm", 3), "brow"], [G2])
            dma("sp", out_d[t0:t0 + 128, :], otF, [G2], [], final=True)
            yield

        def athread(ti):
            yield from gather(ti)
            yield from fstage(ti)

        def inter2(ga, gb_):
            da = db = False
            while not (da and db):
                if not da:
                    try:
                        next(ga)
                    except StopIteration:
                        da = True
                    yield
                if not db:
                    try:
                        next(gb_)
                    except StopIteration:
                        db = True
                    yield

        def run_all(g):
            for _ in g:
                pass

        def interleave(ga, gb_):
            da = db = False
            while not (da and db):
                if not da:
                    try:
                        next(ga)
                    except StopIteration:
                        da = True
                if not db:
                    try:
                        next(gb_)
                    except StopIteration:
                        db = True

        run_all(mixer(0))
        for ti in range(NT):
            if ti + 1 < NT:
                interleave(athread(ti), mixer(ti + 1))
            else:
                run_all(athread(ti))

        if record:
            return seq, None
        S.emit()
    return nc, dbg_out


def pack_inputs(inp):
    f = lambda a: np.ascontiguousarray(np.asarray(a, dtype=np.float32))
    col = lambda v, n: f(v).reshape(n, 128).T
    cv = np.zeros((128, CV_N), np.float32)
    cv[:, CV_NMIX:CV_NMIX + 8] = col(inp["norm_mix_w"][0], 8)
    cv[:, CV_SSDN:CV_SSDN + 16] = col(inp["ssd_norm_w"][0], 16)
    cv[:, CV_NFFN:CV_NFFN + 8] = col(inp["norm_ffn_w"][0], 8)
    cv[:, CV_NPLE:CV_NPLE + 8] = col(inp["norm_ple_w"][0], 8)
    cv[:, CV_LNW:CV_LNW + 8] = col(inp["conv_ln_w"][0], 8)
    cv[:, CV_LNB:CV_LNB + 8] = col(inp["conv_ln_b"][0], 8)
    cv[:, CV_CSSB:CV_CSSB + 32] = col(inp["conv_ssd_b"][0], 32)
    cv[:, CV_CDWB:CV_CDWB + 8] = col(inp["conv_dw_b"][0], 8)
    cv[:, CV_CSSW:CV_CSSW + 128] = f(inp["conv_ssd_w"][0]).reshape(4, 32, 128).transpose(2, 1, 0).reshape(128, 128)
    cv[:, CV_CDWW:CV_CDWW + 248] = f(inp["conv_dw_w"][0]).reshape(31, 8, 128).transpose(2, 1, 0).reshape(128, 248)
    rv = np.zeros((RV_N,), np.float32)
    rv[RV_FNW:RV_FNW + 1024] = f(inp["final_norm_w"])
    rv[RV_NFFN:RV_NFFN + 1024] = f(inp["norm_ffn_w"][0])
    rv[RV_DTB:RV_DTB + 32] = f(inp["dt_bias"][0])
    rv[RV_ALOG:RV_ALOG + 32] = f(inp["a_log"][0])
    rv[RV_DSK:RV_DSK + 32] = f(inp["d_skip"][0])
    rv[RV_BCO:RV_BCO + D] = f(inp["b_conv_out"][0])
    kT = f(inp["peer_keys"][0]).reshape(16, 128, 128).transpose(2, 0, 1).reshape(128, 2048)
    shared = {
        "w_in": f(inp["w_in"][0]), "w_ssd_out": f(inp["w_ssd_out"][0]), "w_conv_out": f(inp["w_conv_out"][0]),
        "w_o": f(inp["w_o"][0]), "peer_wq": f(inp["peer_wq"][0]), "w_ple_gate": f(inp["w_ple_gate"][0]),
        "w_ple_proj": f(inp["w_ple_proj"][0]), "peer_u": f(inp["peer_u"][0]), "peer_v": f(inp["peer_v"][0]),
        "cvec": cv, "rowvec": rv, "kT": np.ascontiguousarray(kT),
    }
    return shared


def kernel(**inputs):
    x = np.asarray(inputs["x"], dtype=np.float32)
    p = np.asarray(inputs["p"], dtype=np.float32)
    B, T, _ = x.shape
    shared = pack_inputs(inputs)
    wseq, _ = build_nc(T)
    nc, _ = build_nc(T, wseq=wseq)
    in_maps = []
    for c in range(B):
        m = dict(shared)
        m["x"] = np.ascontiguousarray(x[c])
        m["p"] = np.ascontiguousarray(p[0, c])
        in_maps.append(m)
    res = run_bass_kernel_spmd(nc, in_maps, core_ids=list(range(B)))
    return np.stack([np.asarray(r["out"], dtype=np.float32) for r in res.results], axis=0)
```

```python
import numpy as np
from contextlib import ExitStack
import concourse.bass as bass
import concourse.mybir as mybir
from concourse.bass_utils import run_bass_kernel_spmd

F32 = mybir.dt.float32
BF16 = mybir.dt.bfloat16
I32 = mybir.dt.int32
U32 = mybir.dt.uint32
AF = mybir.ActivationFunctionType
ALU = mybir.AluOpType
AX = mybir.AxisListType

D = 1024
DI = 2048
INC = 10272
C_Z0, C_XBC0, C_DT0, C_GLUA, C_GLUB, C_GA, C_GB = 0, 2048, 6144, 6176, 7200, 8224, 9248
EPS = 1e-6
CV_NMIX, CV_SSDN, CV_NFFN, CV_NPLE, CV_LNW, CV_LNB, CV_CSSB, CV_CDWB, CV_CSSW, CV_CDWW, CV_N = 0, 8, 24, 32, 40, 48, 56, 88, 96, 224, 472
RV_FNW, RV_NFFN, RV_DTB, RV_ALOG, RV_DSK, RV_BCO, RV_N = 0, 1024, 2048, 2080, 2112, 2144, 3168
NWSLOT = 4


class Sched:
    def __init__(self, nc, stack, n_dma_sems=16, same_engine_sync=("act", "dve", "pool")):
        self.nc = nc
        self.names = ["pe", "act", "dve", "pool", "sp"]
        self.sem = {e: stack.enter_context(nc.semaphore("sem_" + e)) for e in self.names}
        self.cnt = {e: 0 for e in self.names}
        self.prog = {e: [] for e in self.names}
        self.waited = {e: {} for e in self.names}
        self.dsem, self.dcnt, self.dnext = {}, {}, {}
        for q in ("sp", "pool"):
            self.dsem[q] = [stack.enter_context(nc.semaphore(f"dsem_{q}{i}")) for i in range(n_dma_sems)]
            self.dcnt[q] = [0] * n_dma_sems
            self.dnext[q] = 0
        self.last_w = {}
        self.readers = {}
        self.same_engine_sync = same_engine_sync
        self.out_tokens = []

    def _need(self, e, tok):
        key, h, v, pe = tok
        if pe == e and (e == "pe" or e not in self.same_engine_sync):
            return
        if self.waited[e].get(key, 0) >= v:
            return
        self.waited[e][key] = v
        self.prog[e].append(("wait", h, v))

    def op(self, e, fn, reads=(), writes=(), dma=False, final=False):
        deps = []
        for r in reads:
            if r in self.last_w:
                deps.append(self.last_w[r])
        for w in writes:
            if w in self.last_w:
                deps.append(self.last_w[w])
            deps.extend(self.readers.get(w, {}).values())
        for tok in deps:
            self._need(e, tok)
        if dma:
            i = self.dnext[e]
            self.dnext[e] = (i + 1) % len(self.dsem[e])
            self.dcnt[e][i] += 16
            tok = (("d", e, i), self.dsem[e][i], self.dcnt[e][i], "dma")
            self.prog[e].append(("inst", fn, self.dsem[e][i], 16))
        else:
            self.cnt[e] += 1
            tok = (("e", e), self.sem[e], self.cnt[e], e)
            self.prog[e].append(("inst", fn, self.sem[e], 1))
        for r in reads:
            self.readers.setdefault(r, {})[tok[0]] = tok
        for w in writes:
            self.last_w[w] = tok
            self.readers[w] = {}
        if final:
            self.out_tokens.append(tok)
        return tok

    def emit(self):
        nc = self.nc
        for tok in self.out_tokens:
            self._need("sp", tok)
        progs = self.prog

        def run(eng, lst):
            for it in lst:
                if it[0] == "wait":
                    eng.wait_ge(it[1], it[2])
                else:
                    it[1](eng).then_inc(it[2], it[3])

        with nc.Block() as block:
            @block.tensor
            def _(eng):
                run(eng, progs["pe"])

            @block.scalar
            def _(eng):
                run(eng, progs["act"])

            @block.vector
            def _(eng):
                run(eng, progs["dve"])

            @block.gpsimd
            def _(eng):
                run(eng, progs["pool"])

            @block.sync
            def _(eng):
                run(eng, progs["sp"])


def build_nc(T, dbg=(), dbg_tile=0, same_engine_sync=("act", "dve", "pool"), wseq=None):
    NT = T // 128
    nc = bass.Bass("TRN2", target_bir_lowering=False)
    din = lambda n, s, dt=F32: nc.dram_tensor(n, list(s), dt, kind="ExternalInput").ap()
    x_d = din("x", [T, D])
    p_d = din("p", [T, 256])
    wsrc = {
        "w_in": din("w_in", [D, INC]), "w_ssd_out": din("w_ssd_out", [DI, D]),
        "w_conv_out": din("w_conv_out", [D, D]), "w_o": din("w_o", [D, D]),
        "peer_wq": din("peer_wq", [D, 2048]), "w_ple_gate": din("w_ple_gate", [D, D]),
        "w_ple_proj": din("w_ple_proj", [256, D]),
    }
    wsrc["kT"] = din("kT", [128, 2048])
    pu_d = din("peer_u", [16384, D])
    pv_d = din("peer_v", [16384, D])
    cvec_d = din("cvec", [128, CV_N])
    rowvec_d = din("rowvec", [RV_N])
    out_d = nc.dram_tensor("out", [T, D], F32, kind="ExternalOutput").ap()
    wblk = nc.dram_tensor("wblk", [48, 128, 8 * 512], BF16, kind="Internal").ap()
    uv_scr = nc.dram_tensor("uv_scr", [16384, 2048], BF16, kind="Internal").ap()
    dbg_out = {}

    with ExitStack() as st:
        S = Sched(nc, st, same_engine_sync=same_engine_sync)
        sb = lambda name, shape, dt=F32: st.enter_context(nc.sbuf_tensor("sb_" + name, list(shape), dt))
        PSB = [st.enter_context(nc.psum_tensor(f"psb{b}", [128, 512], F32)) for b in range(8)]
        PR = lambda b: ("ps", b)
        bank_ctr = [0]
        bank_lim = [6]

        def nb():
            b = bank_ctr[0] % bank_lim[0]
            bank_ctr[0] = (b + 1) % bank_lim[0]
            return b

        def mm(out, lhsT, rhs, start, stop, r, w):
            S.op("pe", lambda e: e.matmul(out, lhsT=lhsT, rhs=rhs, start=start, stop=stop), reads=r, writes=w)

        def tr(out, in_, ident, r, w):
            S.op("pe", lambda e: e.transpose(out=out, in_=in_, identity=ident), reads=r, writes=w)

        def actf(out, in_, func, r, w, **kw):
            S.op("act", lambda e: e.activation(out=out, in_=in_, func=func, **kw), reads=r, writes=w)

        def tt(eng, out, in0, in1, op, r, w):
            S.op(eng, lambda e: e.tensor_tensor(out=out, in0=in0, in1=in1, op=op), reads=r, writes=w)

        def tsc(eng, out, in0, s1, s2, op0, op1, r, w):
            if s2 is None:
                S.op(eng, lambda e: e.tensor_scalar(out=out, in0=in0, scalar1=s1, scalar2=None, op0=op0), reads=r, writes=w)
            else:
                S.op(eng, lambda e: e.tensor_scalar(out=out, in0=in0, scalar1=s1, scalar2=s2, op0=op0, op1=op1), reads=r, writes=w)

        def tss(eng, out, in_, s, op, r, w):
            S.op(eng, lambda e: e.tensor_single_scalar(out=out, in_=in_, scalar=s, op=op), reads=r, writes=w)

        def stt(eng, out, in0, scalar, in1, op0, op1, r, w):
            S.op(eng, lambda e: e.scalar_tensor_tensor(out=out, in0=in0, scalar=scalar, in1=in1, op0=op0, op1=op1), reads=r, writes=w)

        def cp(eng, out, in_, r, w):
            if eng == "act":
                S.op("act", lambda e: e.copy(out=out, in_=in_), reads=r, writes=w)
            else:
                S.op(eng, lambda e: e.tensor_copy(out=out, in_=in_), reads=r, writes=w)

        def red(eng, out, in_, op, r, w):
            S.op(eng, lambda e: e.tensor_reduce(out=out, in_=in_, axis=AX.X, op=op), reads=r, writes=w)

        def recip(out, in_, r, w):
            S.op("dve", lambda e: e.reciprocal(out=out, in_=in_), reads=r, writes=w)

        def mset(eng, ap, v, w):
            S.op(eng, lambda e: e.memset(ap, v), writes=w)

        def dma(q, out, in_, r, w, final=False):
            S.op(q, lambda e: e.dma_start(out=out, in_=in_), reads=r, writes=w, dma=True, final=final)

        def mkap(t, off, dims):
            a = t[:]
            return bass.AP(a.tensor, off, [list(a.ap[0])] + [list(d) for d in dims])

        def dump(name, ap, shape, r, dt=F32):
            if name in dbg:
                o = nc.dram_tensor("dbg_" + name, list(shape), dt, kind="ExternalOutput").ap()
                dbg_out[name] = o
                dma("sp", o, ap, r, [], final=True)

        identf = sb("identf", [128, 128]); identb = sb("identb", [128, 128], BF16)
        triu = sb("triu", [128, 128]); slt = sb("slt", [128, 128]); onesf = sb("onesf", [128, 128])
        onesb = sb("onesb", [1, 128], BF16)
        triub = sb("triub", [128, 128], BF16)
        iota16 = sb("iota16", [128, 16])
        cvec = sb("cvec", [128, CV_N]); brow = sb("brow", [128, RV_BCO])
        aneg = sb("aneg", [128, 32])
        bcob = sb("bcob", [1, D], BF16)
        state = sb("state", [128, 2048]); state_bf = sb("state_bf", [128, 2048], BF16)
        halo_x = sb("halo_x", [128, 32, 3]); rawb = [sb(f"rawb{i}", [128, 4, 131]) for i in range(2)]
        uraw = sb("uraw", [128, 8, 158])
        wbuf = [sb(f"wbuf{i}", [128, 8, 512], BF16) for i in range(NWSLOT)]
        Fb = [sb(f"F{i}", [128, 2048]) for i in range(4)]
        Hb = [sb(f"H{i}", [128, 2048], BF16) for i in range(4)]
        FR = lambda i: [("F", i, 0), ("F", i, 1)]
        HR = lambda i: [("H", i)]
        xts = [sb(f"xt{i}", [128, D]) for i in range(2)]; xs = sb("xs", [128, D], BF16); hnT = sb("hnT", [128, 8, 128], BF16)
        xsns = [sb(f"xsn{i}", [128, D], BF16) for i in range(2)]
        gbufs = [sb(f"gb{i}", [128, 2048], BF16) for i in range(8)]
        sm = sb("sm", [128, 64])
        dtt = sb("dtt", [128, 32]); la = sb("la", [128, 32]); acum = sb("acum", [128, 32]); decarg = sb("decarg", [128, 32])
        eacum = sb("eacum", [128, 32]); decay = sb("decay", [128, 32]); dA = sb("dA", [128, 32])
        xcT = sb("xcT", [128, 16, 128], BF16); BT = sb("BT", [128, 8, 128], BF16); CT = sb("CT", [128, 8, 128], BF16)
        cacc = [sb(f"cacc{i}", [128, 4, 128]) for i in range(2)]
        x_tok = sb("x_tok", [128, 2048], BF16); B_tok = sb("B_tok", [128, 1024], BF16)
        sig = sb("sig", [128, 512]); sigY = sb("sigY", [128, 512]); uconv = sb("uconv", [128, 8, 128]); uact = sb("uact", [128, 8, 128], BF16)
        lnst = sb("lnst", [128, 3, 128])
        sv = sb("sv", [128, 256]); siu = sb("siu", [128, 256], U32); sif = sb("sif", [128, 256])
        best = sb("best", [128, 128]); ju = sb("ju", [128, 128], U32); jt = sb("jt", [128, 128], U32)
        jaf = sb("jaf", [128, 128]); jbf = sb("jbf", [128, 128]); i1f = sb("i1f", [128, 128]); i2f = sb("i2f", [128, 128])
        eidxs = [sb(f"eidx{i}", [128, 128], I32) for i in range(2)]; gtes = [sb(f"gte{i}", [128, 128]) for i in range(2)]; dots = sb("dots", [128, 128]); wts = sb("wts", [128, 128])
        dg = [sb(f"dg{i}", [128, 128], BF16) for i in range(4)]
        pt = sb("pt", [128, 256]); ptb = sb("ptb", [128, 256], BF16); ptT = sb("ptT", [128, 2, 128], BF16)

        mset("pool", identf[:], 1.0, ["identf"])
        S.op("pool", lambda e: e.affine_select(out=identf[:], in_=identf[:], pattern=[[-1, 128]], compare_op=ALU.is_equal, fill=0.0, base=0, channel_multiplier=1), reads=["identf"], writes=["identf"])
        cp("dve", identb[:], identf[:], ["identf"], ["identb"])
        mset("pool", triu[:], 1.0, ["triu"])
        S.op("pool", lambda e: e.affine_select(out=triu[:], in_=triu[:], pattern=[[1, 128]], compare_op=ALU.is_ge, fill=0.0, base=0, channel_multiplier=-1), reads=["triu"], writes=["triu"])
        cp("dve", triub[:], triu[:], ["triu"], ["triub"])
        mset("pool", slt[:], 1.0, ["slt"])
        S.op("pool", lambda e: e.affine_select(out=slt[:], in_=slt[:], pattern=[[-1, 128]], compare_op=ALU.is_ge, fill=0.0, base=-1, channel_multiplier=1), reads=["slt"], writes=["slt"])
        mset("pool", onesf[:], 1.0, ["onesf"])
        mset("pool", onesb[:], 1.0, ["onesb"])
        for a in range(16):
            mset("pool", iota16[:, a:a + 1], float(a), ["iota16"])
        mset("pool", state[:], 0.0, ["state"])
        mset("pool", state_bf[:], 0.0, ["state_bf"])
        mset("pool", halo_x[:], 0.0, [("halo_x", j) for j in range(8)])
        mset("pool", uraw[:], 0.0, [("uraw", 0), ("uraw", 1)])
        dma("sp", cvec[:], cvec_d, [], ["cvec"])
        dma("sp", brow[:], rowvec_d[0:RV_BCO].partition_broadcast(128), [], ["brow"])
        dma("pool", bcob[:], rowvec_d[RV_BCO:RV_BCO + D].rearrange("(a n) -> a n", a=1), [], ["bcob"])
        actf(aneg[:], brow[:, RV_ALOG:RV_ALOG + 32], AF.Exp, ["brow"], ["aneg"])
        tss("dve", aneg[:], aneg[:], -1.0, ALU.mult, ["aneg"], ["aneg"])

        record = wseq is None
        seq = [] if record else list(wseq)
        wstate = {"issued": 0, "consumed": 0}

        blk_index = {}
        if not record:
            for spec in seq:
                if spec not in blk_index:
                    bi = len(blk_index)
                    blk_index[spec] = bi
                    n, k0, nk, c0, ncol = spec
                    src = wsrc[n][k0 * 128:(k0 + nk) * 128, c0:c0 + ncol].rearrange("(kc p) n -> p kc n", p=128)
                    dst = wblk[bi][:, 0:nk * ncol].rearrange("p (kc n) -> p kc n", kc=nk)
                    dma("pool", dst, src, [], [("blk", bi)])

        uvres = []
        for rc in range(16):
            for hf, src in enumerate((pu_d, pv_d)):
                dma("pool", uv_scr[rc * 1024:(rc + 1) * 1024, hf * 1024:(hf + 1) * 1024], src[rc * 1024:(rc + 1) * 1024, :], [], [("uv", rc, hf)])
                uvres.append(("uv", rc, hf))

        S.op("pool", lambda e: e.memset(sm[:, 60:61], 0.0), reads=uvres, writes=["uv_ready"])

        def w_issue(i):
            n, k0, nk, c0, ncol = seq[i]
            s = i % NWSLOT
            bi = blk_index[seq[i]]
            src = wblk[bi][:, 0:nk * ncol].rearrange("p (kc n) -> p kc n", kc=nk)
            dma("sp", wbuf[s][:, 0:nk, 0:ncol], src, [("blk", bi)], [("wbuf", s)])

        def wnext(expect, k0=0, ncol=512, nk=8):
            spec = (expect[0], k0, nk, expect[1], ncol)
            if record:
                seq.append(spec)
                return wbuf[0], ("wbuf", 0)
            while wstate["issued"] < min(len(seq), wstate["consumed"] + NWSLOT):
                w_issue(wstate["issued"])
                wstate["issued"] += 1
            i = wstate["consumed"]
            wstate["consumed"] += 1
            assert seq[i] == spec, (seq[i], spec)
            s = i % NWSLOT
            return wbuf[s], ("wbuf", s)

        def rmsnorm_stats(src, src_res, col, scr, scr_res):
            actf(scr, src, AF.Square, src_res, scr_res + [("sm", col)], accum_out=sm[:, col:col + 1])
            actf(sm[:, col:col + 1], sm[:, col:col + 1], AF.Sqrt, [("sm", col)], [("sm", col)], scale=1.0 / D, bias=EPS)
            recip(sm[:, col:col + 1], sm[:, col:col + 1], [("sm", col)], [("sm", col)])

        def to_featmajor(src_bf, src_res, dstT, dst_res, nchunk, scale_cols=None, eng="dve", banks=None):
            for b0 in range(0, nchunk, 8):
                b = nb() if banks is None else banks[(b0 // 8) % len(banks)]
                pv = PSB[b][:].bitcast(BF16)
                n = min(8, nchunk - b0)
                for c in range(n):
                    tr(pv[:, c * 128:(c + 1) * 128], src_bf[:, (b0 + c) * 128:(b0 + c + 1) * 128], identb[:], src_res + ["identb"], [PR(b)])
                pv3 = pv[:, 0:n * 128].rearrange("p (a b) -> p a b", a=n)
                if scale_cols is None:
                    cp(eng, dstT[:, b0:b0 + n, :], pv3, [PR(b)], dst_res)
                else:
                    sc_ap = cvec[:, scale_cols + b0:scale_cols + b0 + n].unsqueeze(2).to_broadcast([128, n, 128])
                    tt("dve", dstT[:, b0:b0 + n, :], pv3, sc_ap, ALU.mult, [PR(b), "cvec"], dst_res)

        def mixer(ti):
            t0 = ti * 128
            par = ti % 2
            xt, eidx, gte, xsn = xts[par], eidxs[par], gtes[par], xsns[par]
            XT, EI, GT, XS = ("xt", par), ("eidx", par), ("gte", par), ("xsn", par)
            dbg_on = (ti == dbg_tile)
            D_ = (lambda name, ap, shape, r, dt=F32: dump(name, ap, shape, r, dt)) if dbg_on else (lambda *a, **k: None)
            dma("sp", xt[:], x_d[t0:t0 + 128, :], [], [XT])
            rmsnorm_stats(xt[:], [XT], 0, xs[:], ["xs"])
            actf(xs[:], xt[:], AF.Copy, [XT, ("sm", 0)], ["xs"], scale=sm[:, 0:1])
            to_featmajor(xs, ["xs"], hnT, ["hnT"], 8, scale_cols=CV_NMIX)
            D_("hnT", hnT[:], [128, 8, 128], ["hnT"], BF16)

            flags = {'y': False}
            bank_lim[0] = 4

            def ythread():
                for jj in range(2):
                    ba, bg = 4, 5
                    yield
                    wa, wra = wnext(("w_in", C_GLUA + 512 * jj))
                    for q in range(4):
                        for kc in range(8):
                            mm(PSB[ba][:, q * 128:(q + 1) * 128], wa[:, kc, q * 128:(q + 1) * 128], hnT[:, kc, :], kc == 0, kc == 7, [wra, "hnT"], [PR(ba)])
                    yield
                    wg, wrg = wnext(("w_in", C_GLUB + 512 * jj))
                    for q in range(4):
                        for kc in range(8):
                            mm(PSB[bg][:, q * 128:(q + 1) * 128], wg[:, kc, q * 128:(q + 1) * 128], hnT[:, kc, :], kc == 0, kc == 7, [wrg, "hnT"], [PR(bg)])
                    actf(sigY[:], PSB[bg][:], AF.Sigmoid, [PR(bg)], ["sigY"])
                    tt("dve", uraw[:, 4 * jj:4 * jj + 4, 30:158], PSB[ba][:].rearrange("p (a b) -> p a b", a=4), sigY[:].rearrange("p (a b) -> p a b", a=4), ALU.mult, [PR(ba), "sigY"], [("uraw", jj)])

                for k in range(31):
                    yield
                    for c in range(8):
                        ur = ("uraw", c // 4)
                        wk = cvec[:, CV_CDWW + c * 31 + k:CV_CDWW + c * 31 + k + 1]
                        if k == 0:
                            tsc("dve", uconv[:, c, :], uraw[:, c, 0:128], wk, cvec[:, CV_CDWB + c:CV_CDWB + c + 1], ALU.mult, ALU.add, [ur, "cvec"], [("uconv", c)])
                        else:
                            stt("dve", uconv[:, c, :], uraw[:, c, k:k + 128], wk, uconv[:, c, :], ALU.mult, ALU.add, [ur, "cvec", ("uconv", c)], [("uconv", c)])
                for jj in range(2):
                    cp("act", uraw[:, 4 * jj:4 * jj + 4, 0:30], uraw[:, 4 * jj:4 * jj + 4, 128:158], [("uraw", jj)], [("uraw", jj)])
                ucr = [("uconv", c) for c in range(8)]
                D_("uraw", uraw[:], [128, 8, 158], [("uraw", 0), ("uraw", 1)])
                D_("uconv", uconv[:], [128, 8, 128], ucr)
                yield
                b = 4
                for c in range(8):
                    mm(PSB[b][:, 0:128], onesf[:], uconv[:, c, :], c == 0, c == 7, ["onesf", ("uconv", c)], [PR(b)])
                sq3 = sigY[:].rearrange("p (a b) -> p a b", a=4)
                for h2 in range(2):
                    actf(sq3, uconv[:, 4 * h2:4 * h2 + 4, :], AF.Square, ucr[4 * h2:4 * h2 + 4], ["sigY"])
                    for q in range(4):
                        c = 4 * h2 + q
                        mm(PSB[b][:, 128:256], onesf[:], sq3[:, q, :], c == 0, c == 7, ["onesf", "sigY"], [PR(b)])
                tss("dve", lnst[:, 0, :], PSB[b][:, 0:128], 1.0 / D, ALU.mult, [PR(b)], ["lnst"])
                tss("dve", lnst[:, 1, :], PSB[b][:, 128:256], 1.0 / D, ALU.mult, [PR(b)], ["lnst"])
                tt("dve", lnst[:, 2, :], lnst[:, 0, :], lnst[:, 0, :], ALU.mult, ["lnst"], ["lnst"])
                tt("dve", lnst[:, 1, :], lnst[:, 1, :], lnst[:, 2, :], ALU.subtract, ["lnst"], ["lnst"])
                actf(lnst[:, 1, :], lnst[:, 1, :], AF.Sqrt, ["lnst"], ["lnst"], bias=EPS)
                recip(lnst[:, 1, :], lnst[:, 1, :], ["lnst"], ["lnst"])
                un = uconv
                yield
                tt("dve", un[:], uconv[:], lnst[:, 0:1, :].to_broadcast([128, 8, 128]), ALU.subtract, ucr + ["lnst"], ucr)
                tt("dve", un[:], un[:], lnst[:, 1:2, :].to_broadcast([128, 8, 128]), ALU.mult, ucr + ["lnst"], ucr)
                yield
                for c in range(8):
                    actf(uact[:, c, :], un[:, c, :], AF.Silu, [("uconv", c), "cvec"], ["uact"], scale=cvec[:, CV_LNW + c:CV_LNW + c + 1], bias=cvec[:, CV_LNB + c:CV_LNB + c + 1])
                D_("uact", uact[:], [128, 8, 128], ["uact"], BF16)


                flags['y'] = True

            def xthread():
                for j in range(8):
                    yield
                    wb, wr = wnext(("w_in", C_XBC0 + 512 * j))
                    b = nb()
                    for q in range(4):
                        for kc in range(8):
                            mm(PSB[b][:, q * 128:(q + 1) * 128], wb[:, kc, q * 128:(q + 1) * 128], hnT[:, kc, :], kc == 0, kc == 7, [wr, "hnT"], [PR(b)])
                    rb, rbr = rawb[j % 2], ("rawb", j % 2)
                    cp("act", rb[:, :, 3:131], PSB[b][:].rearrange("p (a b) -> p a b", a=4), [PR(b)], [rbr])
                    cp("act", rb[:, :, 0:3], halo_x[:, 4 * j:4 * j + 4, :], [("halo_x", j)], [rbr])
                    ca = cacc[j % 2]
                    car = ("cacc", j % 2)
                    for k in range(4):
                        for q in range(4):
                            cc = 4 * j + q
                            wk = cvec[:, CV_CSSW + cc * 4 + k:CV_CSSW + cc * 4 + k + 1]
                            if k == 0:
                                tsc("dve", ca[:, q, :], rb[:, q, 0:128], wk, cvec[:, CV_CSSB + cc:CV_CSSB + cc + 1], ALU.mult, ALU.add, [rbr, "cvec"], [(car, q)])
                            else:
                                stt("dve", ca[:, q, :], rb[:, q, k:k + 128], wk, ca[:, q, :], ALU.mult, ALU.add, [rbr, "cvec", (car, q)], [(car, q)])
                    cp("act", halo_x[:, 4 * j:4 * j + 4, :], rb[:, :, 128:131], [rbr], [("halo_x", j)])
                    car = [(car, q) for q in range(4)]
                    if j < 4:
                        dst, dres = xcT[:, 4 * j:4 * j + 4, :], ["xcT"]
                    elif j < 6:
                        dst, dres = BT[:, 4 * (j - 4):4 * (j - 4) + 4, :], ["BT"]
                    else:
                        dst, dres = CT[:, 4 * (j - 6):4 * (j - 6) + 4, :], ["CT"]
                    actf(dst, ca[:], AF.Silu, car, dres)
                D_("xcT", xcT[:], [128, 16, 128], ["xcT"], BF16)
                D_("BT", BT[:], [128, 8, 128], ["BT"], BF16)
                D_("CT", CT[:], [128, 8, 128], ["CT"], BF16)

                yield
                wb, wr = wnext(("w_in", C_DT0), ncol=32)
                b = nb()
                for kc in range(8):
                    mm(PSB[b][:, 0:32], hnT[:, kc, :], wb[:, kc, 0:32], kc == 0, kc == 7, [wr, "hnT"], [PR(b)])
                tt("dve", dtt[:], PSB[b][:, 0:32], brow[:, RV_DTB:RV_DTB + 32], ALU.add, [PR(b), "brow"], ["dtt"])
                actf(dtt[:], dtt[:], AF.Exp, ["dtt"], ["dtt"])
                actf(dtt[:], dtt[:], AF.Ln, ["dtt"], ["dtt"], bias=1.0)
                tt("dve", la[:], dtt[:], aneg[:], ALU.mult, ["dtt", "aneg"], ["la"])
                D_("dt", dtt[:], [128, 32], ["dtt"])


                to_featmajor(xcT[:].rearrange("p a b -> p (a b)"), ["xcT"], x_tok[:].rearrange("p (a b) -> p a b", a=16), ["x_tok"], 16, eng="act")
                to_featmajor(BT[:].rearrange("p a b -> p (a b)"), ["BT"], B_tok[:].rearrange("p (a b) -> p a b", a=8), ["B_tok"], 8, eng="dve")
                yield
                b = nb()
                mm(PSB[b][:, 0:32], triu[:], la[:], True, True, ["triu", "la"], [PR(b)])
                mm(PSB[b][:, 32:64], onesf[:], la[:], True, True, ["onesf", "la"], [PR(b)])
                cp("dve", acum[:], PSB[b][:, 0:32], [PR(b)], ["acum"])
                tt("dve", decarg[:], PSB[b][:, 32:64], acum[:], ALU.subtract, [PR(b), "acum"], ["decarg"])
                actf(eacum[:], acum[:], AF.Exp, ["acum"], ["eacum"])
                actf(decay[:], decarg[:], AF.Exp, ["decarg"], ["decay"])
                actf(dA[:], PSB[b][:, 32:64], AF.Exp, [PR(b)], ["dA"])
                yield
                cbm = Fb[2][:, 0:1024].rearrange("p (a b) -> p a b", a=8)
                for gb_ in range(2):
                    b = gb_
                    for g4 in range(4):
                        g = 4 * gb_ + g4
                        mm(PSB[b][:, g4 * 128:(g4 + 1) * 128], BT[:, g, :], CT[:, g, :], True, True, ["BT", "CT"], [PR(b)])
                    tt("dve", cbm[:, 4 * gb_:4 * gb_ + 4, :], PSB[b][:].rearrange("p (a b) -> p a b", a=4), triu[:].unsqueeze(1).to_broadcast([128, 4, 128]), ALU.mult, [PR(b), "triu"], [("F", 2, 0)])
                yield
                xdt, xdd, xsk = Hb[1], Hb[2], Hb[3]
                x3 = x_tok[:].rearrange("p (h d) -> p h d", h=32)
                tt("dve", xdt[:].rearrange("p (h d) -> p h d", h=32), x3, dtt[:].unsqueeze(2).to_broadcast([128, 32, 64]), ALU.mult, ["x_tok", "dtt"], HR(1))
                tt("dve", xdd[:].rearrange("p (h d) -> p h d", h=32), xdt[:].rearrange("p (h d) -> p h d", h=32), decay[:].unsqueeze(2).to_broadcast([128, 32, 64]), ALU.mult, HR(1) + ["decay"], HR(2))
                tt("dve", xsk[:].rearrange("p (h d) -> p h d", h=32), x3, brow[:, RV_DSK:RV_DSK + 32].unsqueeze(2).to_broadcast([128, 32, 64]), ALU.mult, ["x_tok", "brow"], HR(3))
                Gt, Et, Mt = Fb[0], Fb[1], Hb[0]
                S.op("dve", lambda e: e.memset(sm[:, 63:64], 0.0), reads=[], writes=HR(0) + [("Hm", 0), ("Hm", 1)])
                yield
                for qq in range(4):
                    ph = qq % 2
                    Gq = Gt[:].bitcast(BF16)[:, ph * 2048:ph * 2048 + 1024]
                    Eq = Et[:, ph * 1024:(ph + 1) * 1024]
                    Mq = Mt[:, ph * 1024:(ph + 1) * 1024]
                    tt("dve", Gq.rearrange("p (a b) -> p a b", a=8), slt[:].unsqueeze(1).to_broadcast([128, 8, 128]), la[:, 8 * qq:8 * qq + 8].unsqueeze(2).to_broadcast([128, 8, 128]), ALU.mult, ["slt", "la"], [("F", 0, ph)])
                    for i in range(8):
                        b = i // 4
                        mm(PSB[b][:, (i % 4) * 128:(i % 4 + 1) * 128], Gq[:, i * 128:(i + 1) * 128], triub[:], True, True, [("F", 0, ph), "triub"], [PR(b)])
                    for b in range(2):
                        actf(Eq[:, b * 512:(b + 1) * 512], PSB[b][:], AF.Exp, [PR(b)], [("F", 1, ph)])
                    yield
                    Ev = mkap(Et, ph * 1024, [[512, 2], [128, 4], [1, 128]])
                    Mv = mkap(Mt, ph * 1024, [[512, 2], [128, 4], [1, 128]])
                    cbv = mkap(Fb[2], 2 * qq * 128, [[128, 2], [0, 4], [1, 128]])
                    tt("dve", Mv, Ev, cbv, ALU.mult, [("F", 1, ph), ("F", 2, 0)], [("Hm", ph)])
                    b = 2
                    mm(PSB[b][:], identb[:], xsk[:, qq * 512:(qq + 1) * 512], True, False, ["identb"] + HR(3), [PR(b)])
                    for i8 in range(8):
                        h = 8 * qq + i8
                        mm(PSB[b][:, i8 * 64:(i8 + 1) * 64], Mq[:, i8 * 128:(i8 + 1) * 128], xdt[:, h * 64:(h + 1) * 64], False, i8 == 7, [("Hm", ph)] + HR(1), [PR(b)])
                    cp("act", Fb[3][:, qq * 512:(qq + 1) * 512], PSB[b][:], [PR(b)], [("F", 3, qq // 2)])
                    yield
                S.op("dve", lambda e: e.memset(sm[:, 63:64], 0.0), reads=[("Hm", 0), ("Hm", 1)], writes=HR(0))
                yv = Fb[3]
                for b in range(4):
                    yield
                    for g2 in range(2):
                        g = 2 * b + g2
                        mm(PSB[b][:, g2 * 256:(g2 + 1) * 256], CT[:, g, :], state_bf[:, g * 256:(g + 1) * 256], True, True, ["CT", "state_bf"], [PR(b)])
                    tt("dve", sig[:].rearrange("p (h d) -> p h d", h=8), PSB[b][:].rearrange("p (h d) -> p h d", h=8), eacum[:, 8 * b:8 * b + 8].unsqueeze(2).to_broadcast([128, 8, 64]), ALU.mult, [PR(b), "eacum"], ["sig"])
                    tt("dve", yv[:, b * 512:(b + 1) * 512], yv[:, b * 512:(b + 1) * 512], sig[:], ALU.add, [("F", 3, b // 2), "sig"], [("F", 3, b // 2)])
                tt("dve", state[:].rearrange("p (h d) -> p h d", h=32), state[:].rearrange("p (h d) -> p h d", h=32), dA[:].unsqueeze(2).to_broadcast([128, 32, 64]), ALU.mult, ["state", "dA"], ["state"])
                for b in range(4):
                    yield
                    for g2 in range(2):
                        g = 2 * b + g2
                        mm(PSB[b][:, g2 * 256:(g2 + 1) * 256], B_tok[:, g * 128:(g + 1) * 128], xdd[:, g * 256:(g + 1) * 256], True, True, ["B_tok"] + HR(2), [PR(b)])
                    tt("dve", state[:, b * 512:(b + 1) * 512], state[:, b * 512:(b + 1) * 512], PSB[b][:], ALU.add, ["state", PR(b)], ["state"])
                cp("act", state_bf[:], state[:], ["state"], ["state_bf"])
                D_("yssm", yv[:], [128, 2048], FR(3))

                yz = Fb[2]
                for j in range(4):
                    yield
                    wb, wr = wnext(("w_in", C_Z0 + 512 * j))
                    b = nb()
                    for kc in range(8):
                        mm(PSB[b][:], hnT[:, kc, :], wb[:, kc, :], kc == 0, kc == 7, [wr, "hnT"], [PR(b)])
                    actf(sig[:], PSB[b][:], AF.Silu, [PR(b)], ["sig"])
                    tt("dve", yz[:, j * 512:(j + 1) * 512], yv[:, j * 512:(j + 1) * 512], sig[:], ALU.mult, [("F", 3, j // 2), "sig"], [("F", 2, j // 2)])
                yield
                actf(Fb[1][:], yz[:], AF.Square, FR(2), FR(1))
                red("dve", sm[:, 8:16], Fb[1][:].rearrange("p (g d) -> p g d", g=8), ALU.add, FR(1), [("sm", 8)])
                actf(sm[:, 8:16], sm[:, 8:16], AF.Sqrt, [("sm", 8)], [("sm", 8)], scale=1.0 / 256, bias=EPS)
                recip(sm[:, 8:16], sm[:, 8:16], [("sm", 8)], [("sm", 8)])
                yield
                yn = Hb[1]
                tt("dve", yn[:].rearrange("p (g d) -> p g d", g=8), yz[:].rearrange("p (g d) -> p g d", g=8), sm[:, 8:16].unsqueeze(2).to_broadcast([128, 8, 256]), ALU.mult, FR(2) + [("sm", 8)], HR(1))
                ynT = Hb[2][:].rearrange("p (a b) -> p a b", a=16)
                to_featmajor(yn, HR(1), ynT, HR(2), 16, scale_cols=CV_SSDN)

                while not flags['y']:
                    yield
                bank_lim[0] = 6

            yield from inter2(ythread(), xthread())
            ynT = Hb[2][:].rearrange("p (a b) -> p a b", a=16)
            bya = [nb(), nb()]
            for j in range(2):
                for kh in range(2):
                    yield
                    wb, wr = wnext(("w_ssd_out", 512 * j), k0=8 * kh)
                    for kc in range(8):
                        mm(PSB[bya[j]][:], ynT[:, 8 * kh + kc, :], wb[:, kc, :], kh == 0 and kc == 0, kh == 1 and kc == 7, HR(2) + [wr], [PR(bya[j])])
            byb = [nb(), nb()]
            for j in range(2):
                yield
                wb, wr = wnext(("w_conv_out", 512 * j))
                for c in range(8):
                    mm(PSB[byb[j]][:], uact[:, c, :], wb[:, c, :], c == 0, False, ["uact", wr], [PR(byb[j])])
                mm(PSB[byb[j]][:], onesb[0:1, :], bcob[0:1, j * 512:(j + 1) * 512], False, True, ["onesb", "bcob"], [PR(byb[j])])
            m1 = Fb[0]
            for br, (c0, banks) in enumerate(((C_GA, bya), (C_GB, byb))):
                for j in range(2):
                    yield
                    wb, wr = wnext(("w_in", c0 + 512 * j))
                    b = nb()
                    for kc in range(8):
                        mm(PSB[b][:], hnT[:, kc, :], wb[:, kc, :], kc == 0, kc == 7, [wr, "hnT"], [PR(b)])
                    actf(sig[:], PSB[b][:], AF.Sigmoid, [PR(b)], ["sig"])
                    tt("dve", m1[:, br * 1024 + j * 512:br * 1024 + (j + 1) * 512], PSB[banks[j]][:], sig[:], ALU.mult, [PR(banks[j]), "sig"], [("F", 0, br)])
            mg = Hb[3]
            tt("dve", mg[:, 0:1024], m1[:, 0:1024], m1[:, 1024:2048], ALU.add, FR(0), HR(3))
            D_("merged", mg[:, 0:1024], [128, 1024], HR(3), BF16)
            mgT = Hb[0][:, 0:1024].rearrange("p (a b) -> p a b", a=8)
            to_featmajor(mg, HR(3), mgT, HR(0), 8, eng="act")
            for j in range(2):
                yield
                wb, wr = wnext(("w_o", 512 * j))
                b = nb()
                for kc in range(8):
                    mm(PSB[b][:], mgT[:, kc, :], wb[:, kc, :], kc == 0, kc == 7, HR(0) + [wr], [PR(b)])
                tt("dve", xt[:, j * 512:(j + 1) * 512], xt[:, j * 512:(j + 1) * 512], PSB[b][:], ALU.add, [XT, PR(b)], [XT])
            D_("h1", xt[:], [128, 1024], [XT])

            rmsnorm_stats(xt[:], [XT], 1, xsn[:], [XS])
            stt("dve", xsn[:], xt[:], sm[:, 1:2], brow[:, RV_NFFN:RV_NFFN + 1024], ALU.mult, ALU.mult, [XT, ("sm", 1), "brow"], [XS])
            xnT = Hb[1][:, 0:1024].rearrange("p (a b) -> p a b", a=8)
            to_featmajor(xsn, [XS], xnT, HR(1), 8, eng="act")
            qT = Hb[2][:].rearrange("p (a b) -> p a b", a=16)
            for j in range(4):
                yield
                wb, wr = wnext(("peer_wq", 512 * j))
                b = nb()
                for q in range(4):
                    for kc in range(8):
                        mm(PSB[b][:, q * 128:(q + 1) * 128], wb[:, kc, q * 128:(q + 1) * 128], xnT[:, kc, :], kc == 0, kc == 7, [wr] + HR(1), [PR(b)])
                cp("act", qT[:, 4 * j:4 * j + 4, :], PSB[b][:].rearrange("p (a b) -> p a b", a=4), [PR(b)], HR(2))
            sc = Fb[2]
            for j in range(4):
                yield
                wkb, wkr = wnext(("kT", 512 * j), nk=1)
                b = nb()
                for q in range(4):
                    hc = 4 * j + q
                    mm(PSB[b][:, q * 128:(q + 1) * 128], qT[:, hc, :], wkb[:, 0, q * 128:(q + 1) * 128], True, True, HR(2) + [wkr], [PR(b)])
                cp("act", sc[:, j * 512:(j + 1) * 512], PSB[b][:], [PR(b)], [("F", 2, j // 2)])
            D_("scores", sc[:], [128, 2048], FR(2))
            sc2 = Fb[3]

            def top16_batch(items, n):
                def R(kind, key):
                    return [(kind, key)]
                S.op("dve", lambda e: e.memset(sm[:, 62:63], 0.0), reads=[], writes=FR(3) + [("tks2", k[4]) for k in items])
                for it, (src, src_res, vals, idxs, key) in enumerate(items):
                    S.op("dve", (lambda src, vals: lambda e: e.max(out=vals[:, 0:8], in_=src))(src, vals), reads=src_res, writes=R("tkv0", key))
                yield
                for it, (src, src_res, vals, idxs, key) in enumerate(items):
                    S.op("dve", (lambda src, vals, idxs: lambda e: e.max_index(out=idxs[:, 0:8], in_max=vals[:, 0:8], in_values=src))(src, vals, idxs), reads=src_res + R("tkv0", key), writes=R("tki0", key))
                yield
                for it, (src, src_res, vals, idxs, key) in enumerate(items):
                    s2 = sc2[:, it * n:(it + 1) * n]
                    S.op("dve", (lambda src, vals, s2: lambda e: e.match_replace(out=s2, in_to_replace=vals[:, 0:8], in_values=src, imm_value=-1e30))(src, vals, s2), reads=src_res + R("tkv0", key), writes=R("tks2", key))
                yield
                for it, (src, src_res, vals, idxs, key) in enumerate(items):
                    s2 = sc2[:, it * n:(it + 1) * n]
                    S.op("dve", (lambda vals, s2: lambda e: e.max(out=vals[:, 8:16], in_=s2))(vals, s2), reads=R("tks2", key), writes=R("tkv1", key))
                yield
                for it, (src, src_res, vals, idxs, key) in enumerate(items):
                    s2 = sc2[:, it * n:(it + 1) * n]
                    S.op("dve", (lambda vals, idxs, s2: lambda e: e.max_index(out=idxs[:, 8:16], in_max=vals[:, 8:16], in_values=s2))(vals, idxs, s2), reads=R("tks2", key) + R("tkv1", key), writes=R("tki1", key))
                S.op("dve", lambda e: e.memset(sm[:, 62:63], 0.0), reads=[("tks2", k[4]) for k in items], writes=FR(3))
                yield

            items1 = [(sc[:, hc * 128:(hc + 1) * 128], [("F", 2, hc // 8)], sv[:, hc * 16:(hc + 1) * 16], siu[:, hc * 16:(hc + 1) * 16], ("a", hc)) for hc in range(16)]
            yield from top16_batch(items1, 128)
            SVR = [(k, ("a", hc)) for hc in range(16) for k in ("tkv0", "tkv1")]
            SIR = [(k, ("a", hc)) for hc in range(16) for k in ("tki0", "tki1")]
            cp("dve", sif[:], siu[:], SIR, ["sif"])
            cand = Fb[1]
            c_out = mkap(cand, 0, [[256, 8], [16, 16], [1, 16]])
            c_in0 = mkap(sv, 0, [[32, 8], [1, 16], [0, 16]])
            c_in1 = mkap(sv, 16, [[32, 8], [0, 16], [1, 16]])
            tt("dve", c_out, c_in0, c_in1, ALU.add, SVR, FR(1))
            items2 = [(cand[:, h * 256:(h + 1) * 256], [("F", 1, h // 4)], best[:, h * 16:(h + 1) * 16], ju[:, h * 16:(h + 1) * 16], ("b", h)) for h in range(8)]
            yield from top16_batch(items2, 256)
            BSR = [(k, ("b", h)) for h in range(8) for k in ("tkv0", "tkv1")]
            JUR = [(k, ("b", h)) for h in range(8) for k in ("tki0", "tki1")]
            tss("dve", jt[:], ju[:], 15, ALU.bitwise_and, JUR, ["jt"])
            cp("dve", jbf[:], jt[:], ["jt"], ["jbf"])
            tss("dve", jt[:], ju[:], 4, ALU.logical_shift_right, JUR, ["jt"])
            cp("dve", jaf[:], jt[:], ["jt"], ["jaf"])
            oh = Fb[0]
            for half, (jsel, dst) in enumerate(((jaf, i1f), (jbf, i2f))):
                oh4 = mkap(oh, 0, [[256, 8], [16, 16], [1, 16]])
                io4 = mkap(iota16, 0, [[0, 8], [0, 16], [1, 16]])
                js4 = mkap(jsel, 0, [[16, 8], [1, 16], [0, 16]])
                si4 = mkap(sif, 16 * half, [[32, 8], [0, 16], [1, 16]])
                tt("dve", oh4, io4, js4, ALU.is_equal, ["iota16", "jaf", "jbf"], FR(0))
                tt("dve", oh4, oh4, si4, ALU.mult, FR(0) + ["sif"], FR(0))
                red("dve", dst[:], oh[:].rearrange("p (a b) -> p a b", b=16), ALU.add, FR(0), ["i1f" if half == 0 else "i2f"])
            stt("dve", eidx[:], i1f[:], 128.0, i2f[:], ALU.mult, ALU.add, ["i1f", "i2f"], [EI])
            D_(EI, eidx[:], [128, 128], [EI], I32)
            b3 = best[:].rearrange("p (h k) -> p h k", h=8)
            tt("dve", gte[:].rearrange("p (h k) -> p h k", h=8), b3, mkap(best, 0, [[16, 8], [0, 16]]), ALU.subtract, BSR, [GT])
            actf(gte[:], gte[:], AF.Exp, [GT], [GT])
            red("dve", sm[:, 16:24], gte[:].rearrange("p (h k) -> p h k", h=8), ALU.add, [GT], [("sm", 16)])
            recip(sm[:, 16:24], sm[:, 16:24], [("sm", 16)], [("sm", 16)])
            tt("dve", gte[:].rearrange("p (h k) -> p h k", h=8), gte[:].rearrange("p (h k) -> p h k", h=8), sm[:, 16:24].unsqueeze(2).to_broadcast([128, 8, 16]), ALU.mult, [GT, ("sm", 16)], [GT])
            D_("gate", gte[:], [128, 128], [GT])

            yield

        def gather(ti):
            t0 = ti * 128
            par = ti % 2
            xt, eidx, gte, xsn = xts[par], eidxs[par], gtes[par], xsns[par]
            XT, EI, GT, XS = ("xt", par), ("eidx", par), ("gte", par), ("xsn", par)
            dbg_on = (ti == dbg_tile)
            D_ = (lambda name, ap, shape, r, dt=F32: dump(name, ap, shape, r, dt)) if dbg_on else (lambda *a, **k: None)
            gs = [(gbufs[i][:], ("gb", i)) for i in range(len(gbufs))]
            NGS = len(gs)
            bv = [6, 7]
            for grp in range(32):
                gl = []
                for s8 in range(4):
                    slot = grp * 4 + s8
                    gbuf, gres = gs[slot % NGS]
                    gl.append((slot, gbuf, gres))
                    if s8 % 2 == 1:
                        yield
                    S.op("pool", (lambda gbuf, slot: lambda e: e.indirect_dma_start(out=gbuf, out_offset=None, in_=uv_scr, in_offset=bass.IndirectOffsetOnAxis(ap=eidx[:, slot:slot + 1], axis=0)))(gbuf, slot), reads=[EI, "uv_ready"], writes=[gres], dma=True)
                    S.op("dve", (lambda gbuf, slot: lambda e: e.scalar_tensor_tensor(out=gbuf[:, 0:1024], in0=gbuf[:, 0:1024], scalar=1.0, in1=xsn[:], op0=ALU.mult, op1=ALU.mult, accum_out=dots[:, slot:slot + 1]))(gbuf, slot), reads=[gres, XS], writes=[(gres, "u"), ("dots", slot)])
                g8 = slice(grp * 4, grp * 4 + 4)
                actf(wts[:, g8], dots[:, g8], AF.Gelu, [("dots", grp * 4 + q) for q in range(4)], [("wts", grp)])
                tt("dve", wts[:, g8], wts[:, g8], gte[:, g8], ALU.mult, [("wts", grp), GT], [("wts", grp)])
                yield
                for slot, gbuf, gres in gl:
                    dgt = dg[slot % 4]
                    dres = ("dg", slot % 4)
                    actf(dgt[:], identb[:], AF.Copy, ["identb", ("wts", grp)], [dres], scale=wts[:, slot:slot + 1])
                    for j in range(2):
                        mm(PSB[bv[j]][:], dgt[:], gbuf[:, 1024 + 512 * j:1024 + 512 * (j + 1)], slot == 0, slot == 127, [dres, gres], [PR(bv[j])])
            D_("wts", wts[:], [128, 128], [("wts", g) for g in range(32)])
            for j in range(2):
                tt("dve", xt[:, j * 512:(j + 1) * 512], xt[:, j * 512:(j + 1) * 512], PSB[bv[j]][:], ALU.add, [XT, PR(bv[j])], [XT])
            D_("h2", xt[:], [128, 1024], [XT])


            yield

        def fstage(ti):
            t0 = ti * 128
            par = ti % 2
            xt, eidx, gte, xsn = xts[par], eidxs[par], gtes[par], xsns[par]
            XT, EI, GT, XS = ("xt", par), ("eidx", par), ("gte", par), ("xsn", par)
            dbg_on = (ti == dbg_tile)
            D_ = (lambda name, ap, shape, r, dt=F32: dump(name, ap, shape, r, dt)) if dbg_on else (lambda *a, **k: None)
            G0, G1, G2 = ("gb", 0), ("gb", 1), ("gb", 2)
            xsF = gbufs[0][:, 0:1024]
            hpT = gbufs[0][:, 1024:2048].rearrange("p (a b) -> p a b", a=8)
            sigF = gbufs[1][:].bitcast(F32)[:, 0:512]
            otF = gbufs[2][:].bitcast(F32)
            rmsnorm_stats(xt[:], [XT], 2, xsF, [G0])
            actf(xsF, xt[:], AF.Copy, [XT, ("sm", 2)], [G0], scale=sm[:, 2:3])
            to_featmajor(xsF, [G0], hpT, [G0], 8, scale_cols=CV_NPLE, banks=[6])
            dma("sp", pt[:], p_d[t0:t0 + 128, :], [], ["pt"])
            cp("act", ptb[:], pt[:], ["pt"], ["ptb"])
            to_featmajor(ptb, ["ptb"], ptT, ["ptT"], 2, eng="act", banks=[7])
            for j in range(2):
                yield
                wb, wr = wnext(("w_ple_gate", 512 * j))
                b = 6
                for kc in range(8):
                    mm(PSB[b][:], hpT[:, kc, :], wb[:, kc, :], kc == 0, kc == 7, [G0, wr], [PR(b)])
                actf(sigF, PSB[b][:], AF.Sigmoid, [PR(b)], [G1])
                yield
                wpp, wrp = wnext(("w_ple_proj", 512 * j), nk=2)
                b2 = 7
                for kc in range(2):
                    mm(PSB[b2][:], ptT[:, kc, :], wpp[:, kc, :], kc == 0, kc == 1, ["ptT", wrp], [PR(b2)])
                tt("dve", sigF, sigF, PSB[b2][:], ALU.mult, [G1, PR(b2)], [G1])
                tt("dve", xt[:, j * 512:(j + 1) * 512], xt[:, j * 512:(j + 1) * 512], sigF, ALU.add, [XT, G1], [XT])
            yield
            rmsnorm_stats(xt[:], [XT], 3, xsF, [G0])
            stt("dve", otF, xt[:], sm[:, 3:4], brow[:, RV_FNW:RV_FNW + 1024], ALU.mult, ALU.mult, [XT, ("sm", 3), "brow"], [G2])
            dma("sp", out_d[t0:t0 + 128, :], otF, [G2], [], final=True)
            yield

        def athread(ti):
            yield from gather(ti)
            yield from fstage(ti)

        def inter2(ga, gb_):
            da = db = False
            while not (da and db):
                if not da:
                    try:
                        next(ga)
                    except StopIteration:
                        da = True
                    yield
                if not db:
                    try:
                        next(gb_)
                    except StopIteration:
                        db = True
                    yield

        def run_all(g):
            for _ in g:
                pass

        def interleave(ga, gb_, lead=6):
            da = db = False
            for _ in range(lead):
                try:
                    next(gb_)
                except StopIteration:
                    db = True
                    break
            while not (da and db):
                if not da:
                    try:
                        next(ga)
                    except StopIteration:
                        da = True
                if not db:
                    try:
                        next(gb_)
                    except StopIteration:
                        db = True

        run_all(mixer(0))
        for ti in range(NT):
            if ti + 1 < NT:
                interleave(athread(ti), mixer(ti + 1))
            else:
                run_all(athread(ti))

        if record:
            return seq, None
        S.emit()
    return nc, dbg_out


def pack_inputs(inp):
    f = lambda a: np.ascontiguousarray(np.asarray(a, dtype=np.float32))
    col = lambda v, n: f(v).reshape(n, 128).T
    cv = np.zeros((128, CV_N), np.float32)
    cv[:, CV_NMIX:CV_NMIX + 8] = col(inp["norm_mix_w"][0], 8)
    cv[:, CV_SSDN:CV_SSDN + 16] = col(inp["ssd_norm_w"][0], 16)
    cv[:, CV_NFFN:CV_NFFN + 8] = col(inp["norm_ffn_w"][0], 8)
    cv[:, CV_NPLE:CV_NPLE + 8] = col(inp["norm_ple_w"][0], 8)
    cv[:, CV_LNW:CV_LNW + 8] = col(inp["conv_ln_w"][0], 8)
    cv[:, CV_LNB:CV_LNB + 8] = col(inp["conv_ln_b"][0], 8)
    cv[:, CV_CSSB:CV_CSSB + 32] = col(inp["conv_ssd_b"][0], 32)
    cv[:, CV_CDWB:CV_CDWB + 8] = col(inp["conv_dw_b"][0], 8)
    cv[:, CV_CSSW:CV_CSSW + 128] = f(inp["conv_ssd_w"][0]).reshape(4, 32, 128).transpose(2, 1, 0).reshape(128, 128)
    cv[:, CV_CDWW:CV_CDWW + 248] = f(inp["conv_dw_w"][0]).reshape(31, 8, 128).transpose(2, 1, 0).reshape(128, 248)
    rv = np.zeros((RV_N,), np.float32)
    rv[RV_FNW:RV_FNW + 1024] = f(inp["final_norm_w"])
    rv[RV_NFFN:RV_NFFN + 1024] = f(inp["norm_ffn_w"][0])
    rv[RV_DTB:RV_DTB + 32] = f(inp["dt_bias"][0])
    rv[RV_ALOG:RV_ALOG + 32] = f(inp["a_log"][0])
    rv[RV_DSK:RV_DSK + 32] = f(inp["d_skip"][0])
    rv[RV_BCO:RV_BCO + D] = f(inp["b_conv_out"][0])
    kT = f(inp["peer_keys"][0]).reshape(16, 128, 128).transpose(2, 0, 1).reshape(128, 2048)
    shared = {
        "w_in": f(inp["w_in"][0]), "w_ssd_out": f(inp["w_ssd_out"][0]), "w_conv_out": f(inp["w_conv_out"][0]),
        "w_o": f(inp["w_o"][0]), "peer_wq": f(inp["peer_wq"][0]), "w_ple_gate": f(inp["w_ple_gate"][0]),
        "w_ple_proj": f(inp["w_ple_proj"][0]), "peer_u": f(inp["peer_u"][0]), "peer_v": f(inp["peer_v"][0]),
        "cvec": cv, "rowvec": rv, "kT": np.ascontiguousarray(kT),
    }
    return shared


def kernel(**inputs):
    x = np.asarray(inputs["x"], dtype=np.float32)
    p = np.asarray(inputs["p"], dtype=np.float32)
    B, T, _ = x.shape
    shared = pack_inputs(inputs)
    wseq, _ = build_nc(T)
    nc, _ = build_nc(T, wseq=wseq)
    in_maps = []
    for c in range(B):
        m = dict(shared)
        m["x"] = np.ascontiguousarray(x[c])
        m["p"] = np.ascontiguousarray(p[0, c])
        in_maps.append(m)
    res = run_bass_kernel_spmd(nc, in_maps, core_ids=list(range(B)))
    return np.stack([np.asarray(r["out"], dtype=np.float32) for r in res.results], axis=0)
```
